# Optimizing a Trainium2 kernel written in Bass

```python
import math
import jax, jax.numpy as jnp
from jax import lax
import numpy as np

D_MODEL = 1024
BATCH = 2
SEQ = 16384
DEPTH = 2

N_MIXERS = 2
Q_BLOCK = 128
ROPE_THETA = 10000.0
NORM_EPS = 1e-6

DIFF_HEAD_DIM = 64
DIFF_HEADS = D_MODEL // (2 * DIFF_HEAD_DIM)
DIFF_QK_WIDTH = DIFF_HEADS * 2 * DIFF_HEAD_DIM
DIFF_V_DIM = 2 * DIFF_HEAD_DIM
DIFF_V_WIDTH = DIFF_HEADS * DIFF_V_DIM

MLA_NOPE = 128
MLA_ROPE = 64
MLA_QK_DIM = MLA_NOPE + MLA_ROPE
MLA_V = 128
MLA_HEADS = D_MODEL // MLA_V
MLA_Q_LORA = 3 * D_MODEL // 8
MLA_KV_LORA = D_MODEL // 4
MLA_A_WIDTH = MLA_Q_LORA + MLA_KV_LORA + MLA_ROPE

N_GROUPS = 8
EXPERTS_PER_GROUP = 8
N_EXPERTS = N_GROUPS * EXPERTS_PER_GROUP
TOP_K = 2
EXPERT_FF = D_MODEL // 4

kernel_name = "hybrid_diffattn_mla_hiermoe"


def rms_norm(x, gain):
    xf = x.astype(jnp.float32)
    y = xf * lax.rsqrt(jnp.mean(xf * xf, axis=-1, keepdims=True) + NORM_EPS)
    return (y * gain.astype(jnp.float32)).astype(x.dtype)


def rope(x, pos):
    d = x.shape[-1]
    half = d // 2
    inv_freq = 1.0 / (ROPE_THETA ** (jnp.arange(0, d, 2, dtype=jnp.float32) / d))
    ang = pos.astype(jnp.float32)[:, None] * inv_freq[None, :]
    shape = (pos.shape[0],) + (1,) * (x.ndim - 3) + (half,)
    cos = jnp.cos(ang).reshape(shape)
    sin = jnp.sin(ang).reshape(shape)
    xf = x.astype(jnp.float32)
    x1, x2 = xf[..., :half], xf[..., half:]
    return jnp.concatenate([x1 * cos - x2 * sin, x2 * cos + x1 * sin], axis=-1).astype(x.dtype)


def causal_softmax(scores, blk, seq):
    qpos = blk * Q_BLOCK + jnp.arange(Q_BLOCK)
    mask = jnp.arange(seq)[None, :] <= qpos[:, None]
    return jax.nn.softmax(jnp.where(mask, scores, -jnp.inf), axis=-1)


def query_block_map(fn, *qs):
    b, s = qs[0].shape[:2]
    nb = s // Q_BLOCK
    blocks = tuple(q.reshape(b, nb, Q_BLOCK, *q.shape[2:]).swapaxes(0, 1) for q in qs)
    out = lax.map(lambda a: fn(a[0], *a[1:]), (jnp.arange(nb), *blocks))
    return out.swapaxes(0, 1).reshape(b, s, *out.shape[3:])


def diff_attention(h, w_in, q_gain, k_gain, lq1, lk1, lq2, lk2, subln, w_out, lambda_init, pos):
    b, s, _ = h.shape
    qkv = h @ w_in
    q, k, v = jnp.split(qkv, [DIFF_QK_WIDTH, 2 * DIFF_QK_WIDTH], axis=-1)
    q = rope(rms_norm(q.reshape(b, s, DIFF_HEADS, 2, DIFF_HEAD_DIM), q_gain), pos)
    k = rope(rms_norm(k.reshape(b, s, DIFF_HEADS, 2, DIFF_HEAD_DIM), k_gain), pos)
    v = v.reshape(b, s, DIFF_HEADS, DIFF_V_DIM)
    q1, q2 = q[..., 0, :], q[..., 1, :]
    k1, k2 = k[..., 0, :], k[..., 1, :]
    lam = (jnp.exp(jnp.sum(lq1.astype(jnp.float32) * lk1.astype(jnp.float32)))
           - jnp.exp(jnp.sum(lq2.astype(jnp.float32) * lk2.astype(jnp.float32)))
           + lambda_init)
    scale = DIFF_HEAD_DIM ** -0.5

    def block(blk, qb1, qb2):
        s1 = jnp.einsum('bqhd,bkhd->bhqk', qb1, k1).astype(jnp.float32) * scale
        s2 = jnp.einsum('bqhd,bkhd->bhqk', qb2, k2).astype(jnp.float32) * scale
        a = causal_softmax(s1, blk, s) - lam * causal_softmax(s2, blk, s)
        return jnp.einsum('bhqk,bkhd->bqhd', a.astype(v.dtype), v)

    o = query_block_map(block, q1, q2)
    o = rms_norm(o, subln) * (1.0 - lambda_init)
    return o.reshape(b, s, DIFF_V_WIDTH) @ w_out


def mla_attention(h, w_a, q_a_gain, kv_a_gain, w_qb, w_kvb, q_gain, k_gain, w_out, pos):
    b, s, _ = h.shape
    a = h @ w_a
    cq, ckv, k_pe = jnp.split(a, [MLA_Q_LORA, MLA_Q_LORA + MLA_KV_LORA], axis=-1)
    q = (rms_norm(cq, q_a_gain) @ w_qb).reshape(b, s, MLA_HEADS, MLA_QK_DIM)
    kv = (rms_norm(ckv, kv_a_gain) @ w_kvb).reshape(b, s, MLA_HEADS, MLA_NOPE + MLA_V)
    k_nope, v = kv[..., :MLA_NOPE], kv[..., MLA_NOPE:]
    k_pe = jnp.broadcast_to(k_pe[:, :, None, :], (b, s, MLA_HEADS, MLA_ROPE))
    k = jnp.concatenate([k_nope, k_pe], axis=-1)
    q = rms_norm(q, q_gain)
    k = rms_norm(k, k_gain)
    q = jnp.concatenate([q[..., :MLA_NOPE], rope(q[..., MLA_NOPE:], pos)], axis=-1)
    k = jnp.concatenate([k[..., :MLA_NOPE], rope(k[..., MLA_NOPE:], pos)], axis=-1)
    scale = MLA_QK_DIM ** -0.5

    def block(blk, qb):
        sc = jnp.einsum('bqhd,bkhd->bhqk', qb, k).astype(jnp.float32) * scale
        p = causal_softmax(sc, blk, s)
        return jnp.einsum('bhqk,bkhd->bqhd', p.astype(v.dtype), v)

    o = query_block_map(block, q)
    return o.reshape(b, s, MLA_HEADS * MLA_V) @ w_out


def hier_moe(h, w_group, b_group, w_expert, b_expert, w_gate, w_up, w_down):
    b, s, d = h.shape
    t = h.reshape(-1, d)
    n_tok = t.shape[0]
    p_group = jax.nn.softmax((t @ w_group + b_group).astype(jnp.float32), axis=-1)
    g_w, g_idx = lax.top_k(p_group, 1)
    e_logits = (t @ w_expert + b_expert).astype(jnp.float32).reshape(n_tok, N_GROUPS, EXPERTS_PER_GROUP)
    e_logits = jnp.take_along_axis(e_logits, g_idx[:, :, None], axis=1)[:, 0]
    e_w, e_idx = lax.top_k(jax.nn.softmax(e_logits, axis=-1), TOP_K)
    weights = g_w * (e_w / jnp.sum(e_w, axis=-1, keepdims=True))
    expert_id = g_idx * EXPERTS_PER_GROUP + e_idx
    combine = jnp.einsum('tk,tke->te', weights,
                         jax.nn.one_hot(expert_id, N_EXPERTS, dtype=jnp.float32)).astype(t.dtype)

    def body(e, acc):
        hid = jax.nn.silu(t @ w_gate[e]) * (t @ w_up[e])
        return acc + combine[:, e, None] * (hid @ w_down[e])

    out = lax.fori_loop(0, N_EXPERTS, body, jnp.zeros_like(t))
    return out.reshape(b, s, d)


def setup_inputs(seed: int = 0) -> dict:
    key = jax.random.key(seed)
    ks = iter(jax.random.split(key, 40))
    nd = (DEPTH + 1) // 2
    nm = DEPTH // 2

    def nrm(shape, scale):
        return jax.random.normal(next(ks), shape, jnp.float32) * scale

    def gain(shape):
        return 1.0 + nrm(shape, 0.05)

    D = D_MODEL
    return {
        "x": nrm((BATCH, SEQ, D), 1.0),
        "attn_norm": gain((DEPTH, D)),
        "ffn_norm": gain((DEPTH, D)),
        "diff_w_in": nrm((nd, D, 2 * DIFF_QK_WIDTH + DIFF_V_WIDTH), D ** -0.5),
        "diff_q_norm": gain((nd, DIFF_HEAD_DIM)),
        "diff_k_norm": gain((nd, DIFF_HEAD_DIM)),
        "diff_lambda_q1": nrm((nd, DIFF_HEAD_DIM), 0.1),
        "diff_lambda_k1": nrm((nd, DIFF_HEAD_DIM), 0.1),
        "diff_lambda_q2": nrm((nd, DIFF_HEAD_DIM), 0.1),
        "diff_lambda_k2": nrm((nd, DIFF_HEAD_DIM), 0.1),
        "diff_subln": gain((nd, DIFF_V_DIM)),
        "diff_w_out": nrm((nd, DIFF_V_WIDTH, D), DIFF_V_WIDTH ** -0.5),
        "mla_w_a": nrm((nm, D, MLA_A_WIDTH), D ** -0.5),
        "mla_q_a_norm": gain((nm, MLA_Q_LORA)),
        "mla_kv_a_norm": gain((nm, MLA_KV_LORA)),
        "mla_w_qb": nrm((nm, MLA_Q_LORA, MLA_HEADS * MLA_QK_DIM), MLA_Q_LORA ** -0.5),
        "mla_w_kvb": nrm((nm, MLA_KV_LORA, MLA_HEADS * (MLA_NOPE + MLA_V)), MLA_KV_LORA ** -0.5),
        "mla_q_norm": gain((nm, MLA_QK_DIM)),
        "mla_k_norm": gain((nm, MLA_QK_DIM)),
        "mla_w_out": nrm((nm, MLA_HEADS * MLA_V, D), (MLA_HEADS * MLA_V) ** -0.5),
        "moe_w_group": nrm((DEPTH, D, N_GROUPS), D ** -0.5),
        "moe_b_group": nrm((DEPTH, N_GROUPS), 0.01),
        "moe_w_expert": nrm((DEPTH, D, N_EXPERTS), D ** -0.5),
        "moe_b_expert": nrm((DEPTH, N_EXPERTS), 0.01),
        "moe_w_gate": nrm((DEPTH, N_EXPERTS, D, EXPERT_FF), D ** -0.5),
        "moe_w_up": nrm((DEPTH, N_EXPERTS, D, EXPERT_FF), D ** -0.5),
        "moe_w_down": nrm((DEPTH, N_EXPERTS, EXPERT_FF, D), EXPERT_FF ** -0.5),
    }


def reference(x, attn_norm, ffn_norm, diff_w_in, diff_q_norm, diff_k_norm,
              diff_lambda_q1, diff_lambda_k1, diff_lambda_q2, diff_lambda_k2,
              diff_subln, diff_w_out, mla_w_a, mla_q_a_norm, mla_kv_a_norm,
              mla_w_qb, mla_w_kvb, mla_q_norm, mla_k_norm, mla_w_out,
              moe_w_group, moe_b_group, moe_w_expert, moe_b_expert,
              moe_w_gate, moe_w_up, moe_w_down):
    pos = jnp.arange(x.shape[1])
    for i in range(DEPTH):
        h = rms_norm(x, attn_norm[i])
        j = i // N_MIXERS
        if i % N_MIXERS == 0:
            lambda_init = 0.8 - 0.6 * math.exp(-0.3 * i)
            x = x + diff_attention(h, diff_w_in[j], diff_q_norm[j], diff_k_norm[j],
                                   diff_lambda_q1[j], diff_lambda_k1[j],
                                   diff_lambda_q2[j], diff_lambda_k2[j],
                                   diff_subln[j], diff_w_out[j], lambda_init, pos)
        else:
            x = x + mla_attention(h, mla_w_a[j], mla_q_a_norm[j], mla_kv_a_norm[j],
                                  mla_w_qb[j], mla_w_kvb[j], mla_q_norm[j], mla_k_norm[j],
                                  mla_w_out[j], pos)
        h = rms_norm(x, ffn_norm[i])
        x = x + hier_moe(h, moe_w_group[i], moe_b_group[i], moe_w_expert[i], moe_b_expert[i],
                         moe_w_gate[i], moe_w_up[i], moe_w_down[i])
    return x
```

```python
import math
from contextlib import ExitStack
import numpy as np
import ml_dtypes
import concourse.bass as bass
import concourse.mybir as mybir
from concourse.bass_utils import run_bass_kernel_spmd

F32 = mybir.dt.float32
BF16 = mybir.dt.bfloat16
ALU = mybir.AluOpType
AF = mybir.ActivationFunctionType
AX = mybir.AxisListType

D = 1024
NH = 8
NEXP = 64
FF = 256
EPS = 1e-6
NCORES = 8
ST_MAX = 1024
CC_ROWS = 256


class _Op:
    __slots__ = ("eng", "fn", "deps", "dma", "semkey", "dmaval", "signal", "signo", "idx", "dmainc")


class Prog:
    COMPUTE = ("pe", "act", "dve", "pool")
    STREAMS = ("pe", "act", "dve", "pool", "sp")

    def __init__(self):
        self.ops = []
        self.lastw = {}
        self.readers = {}
        self.dma_cnt = {}
        self.last_on = {}
        self.dma_since_barrier = []

    def op(self, eng, fn, r=(), w=(), semkey=None, ndma=1, dmainc=16):
        o = _Op()
        o.dmainc = dmainc
        o.eng = eng
        o.fn = fn
        o.dma = semkey is not None
        o.semkey = semkey
        o.signal = False
        o.signo = 0
        o.dmaval = 0
        o.idx = len(self.ops)
        deps = set()
        for x in r:
            lw = self.lastw.get(x)
            if lw is not None:
                deps.add(lw)
        for x in w:
            lw = self.lastw.get(x)
            if lw is not None:
                deps.add(lw)
            rd = self.readers.get(x)
            if rd:
                for v in rd[0].values():
                    deps.add(v)
                for v in rd[1]:
                    deps.add(v)
        o.deps = deps
        for x in w:
            self.lastw[x] = o
            self.readers[x] = ({}, [])
        for x in r:
            rd = self.readers.get(x)
            if rd is None:
                rd = ({}, [])
                self.readers[x] = rd
            if o.dma:
                rd[1].append(o)
            else:
                rd[0][eng] = o
        if o.dma:
            c = self.dma_cnt.get(semkey, 0) + ndma
            self.dma_cnt[semkey] = c
            o.dmaval = dmainc * c
            self.dma_since_barrier.append(o)
        else:
            self.last_on[eng] = o
        self.ops.append(o)
        return o

    def barrier(self):
        deps = set(self.last_on.values()) | set(self.dma_since_barrier)
        self.dma_since_barrier = []
        for e in self.STREAMS:
            o = _Op()
            o.dmainc = 16
            o.eng = e
            o.fn = None
            o.dma = False
            o.semkey = None
            o.signal = False
            o.signo = 0
            o.dmaval = 0
            o.idx = len(self.ops)
            o.deps = set(deps)
            self.ops.append(o)
        self.lastw = {}
        self.readers = {}

    def emit(self, nc, stack):
        for o in self.ops:
            for d in o.deps:
                if d.dma:
                    continue
                if d.eng == "pe" and o.eng == "pe" and not o.dma:
                    continue
                d.signal = True
        cnt = {e: 0 for e in self.COMPUTE}
        for o in self.ops:
            if o.signal:
                cnt[o.eng] += 1
                o.signo = cnt[o.eng]
        sems = {e: stack.enter_context(nc.semaphore("s_" + e)) for e in self.COMPUTE}
        dsem = {}
        for k in self.dma_cnt:
            dsem[k] = stack.enter_context(nc.semaphore("d_%d" % len(dsem)))
        per = {e: [] for e in self.STREAMS}
        for o in self.ops:
            per[o.eng].append(o)
        block = stack.enter_context(nc.Block())

        def run(stream, eng):
            waited = {}
            for o in per[stream]:
                need = {}
                for d in o.deps:
                    if d.dma:
                        key = ("d", d.semkey)
                        val = d.dmaval
                    else:
                        if d.eng == "pe" and o.eng == "pe" and not o.dma:
                            continue
                        key = ("c", d.eng)
                        val = d.signo
                    if val > need.get(key, 0):
                        need[key] = val
                for key, val in need.items():
                    if waited.get(key, 0) >= val:
                        continue
                    waited[key] = val
                    s = dsem[key[1]] if key[0] == "d" else sems[key[1]]
                    eng.wait_ge(s, val)
                if o.fn is None:
                    continue
                ins = o.fn(eng)
                if o.dma:
                    if isinstance(ins, (list, tuple)):
                        for i_ in ins:
                            i_.then_inc(dsem[o.semkey], o.dmainc)
                    else:
                        ins.then_inc(dsem[o.semkey], o.dmainc)
                elif o.signal:
                    if isinstance(ins, (list, tuple)):
                        ins = ins[-1]
                    ins.then_inc(sems[o.eng], 1)

        @block.sync
        def _(e):
            run("sp", e)

        @block.tensor
        def _(e):
            run("pe", e)

        @block.scalar
        def _(e):
            run("act", e)

        @block.vector
        def _(e):
            run("dve", e)

        @block.gpsimd
        def _(e):
            run("pool", e)


class Arena:
    def __init__(self, ap, ncols):
        self.ap = ap
        self.ncols = ncols
        self.base = 0
        self.cur = 0

    def mark(self):
        self.base = self.cur

    def reset(self):
        self.cur = self.base

    def get(self, ncols, dt=BF16):
        if dt == F32:
            self.cur = (self.cur + 1) // 2 * 2
            n = 2 * ncols
        else:
            n = ncols
        n = (n + 1) // 2 * 2
        a = self.ap[:, self.cur:self.cur + n]
        self.cur += n
        assert self.cur <= self.ncols, ("SBUF arena overflow", self.cur, self.ncols)
        if dt == F32:
            return a.bitcast(F32)
        return a


def build_program(S):
    NKB = S // 128
    NT = S // 512
    SO = S // 4
    NTO = SO // 512
    NG = NTO
    ST = min(ST_MAX, SO)
    nc = bass.Bass("TRN2", target_bir_lowering=False)
    P = Prog()

    def din(name, shape, dt=F32):
        return nc.dram_tensor(name, list(shape), dt, kind="ExternalInput").ap()

    def dscr(name, shape, dt=BF16):
        return nc.dram_tensor(name, list(shape), dt, kind="Internal").ap()

    xb_in = din("xb", [S, D])
    xo_in = din("xo", [SO, D])
    y_out = nc.dram_tensor("y", [SO, D], F32, kind="ExternalOutput").ap()
    X1 = dscr("X1", [SO, D], F32)
    XG = dscr("XG", [4 * SO, D], F32)
    cosk = din("cosk", [128, S])
    sink = din("sink", [128, S])
    cosq = din("cosq", [128, SO])
    sinq = din("sinq", [128, SO])
    maskd = din("maskd", [128, 16 * 512], BF16)
    identb_d = din("identb", [128, 128], BF16)
    identf_d = din("identf", [128, 128])
    onesbd_d = din("onesbd", [128, 128], BF16)
    ones_d = din("ones", [128, 128], BF16)
    rot_d = din("rot", [128, 128], BF16)
    g_attn_all = din("g_attn", [128, 16])
    g_ffn_all = din("g_ffn", [128, 16])
    LW = {}
    for kd in (0, 1):
        pf = "L%d_" % kd
        LW[kd] = dict(
            w_out=din(pf + "w_out", [D, D]), wr=din(pf + "wr", [D, 72]), br=din(pf + "br", [1, 72]),
            w_gate=din(pf + "w_gate", [NEXP, D, FF]), w_up=din(pf + "w_up", [NEXP, D, FF]),
            w_down=din(pf + "w_down", [NEXP, FF, D]))
    w_in = din("w_in", [D, 3 * D])
    gq = din("gq", [128, 1])
    gk = din("gk", [128, 1])
    lam4 = din("lam4", [1, 256])
    subln = din("subln", [1, 128])
    w_a = din("w_a", [D, 704])
    g_qa = din("g_qa", [128, 3])
    g_kva = din("g_kva", [128, 2])
    w_qb = din("w_qb", [384, 1536])
    w_kvb = din("w_kvb", [256, 2048])
    gqn = din("gqn", [128, 1])
    gqr = din("gqr", [64, 1])
    gkn = din("gkn", [128, 1])
    gkr = din("gkr", [64, 1])
    KT = dscr("KT", [NH, 128, S])
    KR = dscr("KR", [NH, 64, S])
    QT = dscr("QT", [NH, 128, SO])
    QR = dscr("QR", [NH, 64, SO])
    VS = dscr("VS", [NH, 128, NKB, 128])
    AT = dscr("AT", [NH, 128, SO])

    stack = ExitStack()
    with stack:
        ARENA_COLS = 94 * 1024
        arena_t = stack.enter_context(nc.sbuf_tensor("arena", [128, ARENA_COLS], BF16))
        A = Arena(arena_t[:], ARENA_COLS)
        ps_t = stack.enter_context(nc.psum_tensor("ps", [128, 4096], F32))
        ps = ps_t[:]

        def bank(i, n=512, off=0):
            return ps[:, i * 512 + off:i * 512 + off + n]

        def bank_bf(i):
            return ps[:, i * 512:(i + 1) * 512].bitcast(BF16)

        identb = A.get(128)
        identf = A.get(128, F32)
        onesbd = A.get(128)
        ones = A.get(128)
        rot = A.get(128)
        gat_all = A.get(16, F32)
        gff_all = A.get(16, F32)
        junk = A.get(1024)
        small = A.get(64, F32)
        P.op("sp", lambda e: [
            e.dma_start(out=identb, in_=identb_d[:, :]),
            e.dma_start(out=identf, in_=identf_d[:, :]),
            e.dma_start(out=onesbd, in_=onesbd_d[:, :]),
            e.dma_start(out=ones, in_=ones_d[:, :]),
            e.dma_start(out=rot, in_=rot_d[:, :]),
            e.dma_start(out=gat_all, in_=g_attn_all[:, :]),
            e.dma_start(out=gff_all, in_=g_ffn_all[:, :]),
        ], w=["const"], semkey="const", ndma=7)
        A.mark()

        def emit_layer(kind, xb, xb_gathered, xo, y):
            gat = gat_all[:, kind * 8:(kind + 1) * 8]
            gff = gff_all[:, kind * 8:(kind + 1) * 8]
            w_out, wr, br = LW[kind]["w_out"], LW[kind]["wr"], LW[kind]["br"]
            w_gate, w_up, w_down = LW[kind]["w_gate"], LW[kind]["w_up"], LW[kind]["w_down"]
            def load_x_tile(src, t, xt, tag, extra_w=(), gathered=False):
                if gathered:
                    k_, o_ = (t * 128) // CC_ROWS, (t * 128) % CC_ROWS
                    in_ap = src.rearrange("(k j n) d -> k j n d", j=4, n=CC_ROWS)[k_, :, o_:o_ + 128, :].rearrange(
                        "j p d -> p j d")
                else:
                    in_ap = src[t * 512:(t + 1) * 512, :].rearrange("(j p) d -> p j d", p=128)
                P.op("sp", lambda e: e.dma_start(
                    out=xt.rearrange("p (j d) -> p j d", j=4), in_=in_ap),
                    w=[tag] + list(extra_w), semkey=tag)

            def norm_tile(xt, xtag, hb, hbtag, st4, sttag):
                for j in range(4):
                    P.op("act", lambda e, j=j: e.activation(
                        out=junk, in_=xt[:, j * 1024:(j + 1) * 1024], func=AF.Square,
                        accum_out=st4[:, j:j + 1]), r=[xtag, "const"], w=["junk", (sttag, j)])
                P.op("act", lambda e: e.activation(out=st4[:, 4:8], in_=st4[:, 0:4], func=AF.Ln,
                                                   scale=1.0 / D, bias=EPS),
                     r=[(sttag, j) for j in range(4)], w=[(sttag, "ln")])
                P.op("act", lambda e: e.activation(out=st4[:, 8:12], in_=st4[:, 4:8], func=AF.Exp,
                                                   scale=-0.5),
                     r=[(sttag, "ln")], w=[(sttag, "rs")])
                for j in range(4):
                    P.op("dve", lambda e, j=j: e.tensor_scalar(
                        out=hb[:, j * 1024:(j + 1) * 1024], in0=xt[:, j * 1024:(j + 1) * 1024],
                        scalar1=st4[:, 8 + j:9 + j], scalar2=None, op0=ALU.mult),
                        r=[xtag, (sttag, "rs")], w=[(hbtag, j)])

            def transpose_tile_bf(hb, hbtag, hT, hTtag, gains, psb):
                for c in range(8):
                    pb = psb[c % 2]
                    pt = bank_bf(pb)[:, 0:512]
                    P.op("pe", lambda e, c=c, pt=pt: [
                        e.transpose(out=pt[:, j * 128:(j + 1) * 128],
                                    in_=hb[:, j * 1024 + c * 128:j * 1024 + (c + 1) * 128],
                                    identity=identb) for j in range(4)],
                        r=[(hbtag, j) for j in range(4)] + ["const"], w=[("ps", pb)])
                    eng = "act" if c % 2 == 0 else "dve"
                    if eng == "act":
                        P.op("act", lambda e, c=c, pt=pt: e.activation(
                            out=hT[c], in_=pt, func=AF.Copy, scale=gains[:, c:c + 1]),
                            r=[("ps", pb), "const"], w=[(hTtag, c)])
                    else:
                        P.op("dve", lambda e, c=c, pt=pt: e.tensor_scalar(
                            out=hT[c], in0=pt, scalar1=gains[:, c:c + 1], scalar2=None, op0=ALU.mult),
                            r=[("ps", pb), "const"], w=[(hTtag, c)])

            def load_cast_weight(dst, dsttag, src_rows, nrows_chunks, ncols, stg, stgtag, col0=0,
                                 engs=("pool", "dve")):
                for c in range(nrows_chunks):
                    s = stg[c % len(stg)]
                    stag = (stgtag, c % len(stg))
                    P.op("sp", lambda e, c=c, s=s: e.dma_start(
                        out=s[:, 0:ncols], in_=src_rows[c * 128:(c + 1) * 128, col0:col0 + ncols]),
                        w=[stag], semkey=stag)
                    eng = engs[c % len(engs)]
                    P.op(eng, lambda e, c=c, s=s: e.tensor_copy(
                        out=dst[:, c * ncols:(c + 1) * ncols], in_=s[:, 0:ncols]),
                        r=[stag], w=[(dsttag, c)])

            def feature_head_post(psrc, pb_src, nparts, g_col, cos_t, sin_t, tabtag, out_bf, outtag,
                                  rstd, rstdtag, tmp, tmptag, psrot):
                kgb, t1, t2 = tmp
                P.op("act", lambda e: e.activation(out=kgb[0:nparts, :], in_=psrc, func=AF.Copy,
                                                   scale=g_col),
                     r=[("ps", pb_src), "const", "gains"], w=[(tmptag, "kgb")])
                pr = bank(psrot)[0:nparts, :]
                P.op("pe", lambda e: e.matmul(pr, lhsT=rot[0:nparts, 0:nparts], rhs=kgb[0:nparts, :],
                                              start=True, stop=True),
                     r=[(tmptag, "kgb"), "const"], w=[("ps", psrot)])
                P.op("dve", lambda e: e.tensor_tensor(out=t1[0:nparts, :], in0=kgb[0:nparts, :],
                                                      in1=cos_t[0:nparts, :], op=ALU.mult),
                     r=[(tmptag, "kgb"), tabtag], w=[(tmptag, "t1")])
                P.op("dve", lambda e: e.tensor_tensor(out=t2[0:nparts, :], in0=pr,
                                                      in1=sin_t[0:nparts, :], op=ALU.mult),
                     r=[("ps", psrot), tabtag], w=[(tmptag, "t2")])
                if rstd is None:
                    P.op("pool", lambda e: e.tensor_tensor(out=out_bf, in0=t1[0:nparts, :],
                                                           in1=t2[0:nparts, :], op=ALU.add),
                         r=[(tmptag, "t2"), (tmptag, "t1")], w=[outtag])
                    return
                P.op("pool", lambda e: e.tensor_tensor(out=t1[0:nparts, :], in0=t1[0:nparts, :],
                                                       in1=t2[0:nparts, :], op=ALU.add),
                     r=[(tmptag, "t2"), (tmptag, "t1")], w=[(tmptag, "t1")])
                P.op("pool", lambda e: e.tensor_tensor(out=out_bf, in0=t1[0:nparts, :],
                                                       in1=rstd[0:nparts, :], op=ALU.mult),
                     r=[(tmptag, "t1"), rstdtag], w=[outtag])

            def rstd_from_ps(psms, pb, rstd, rstdtag, lnt, n):
                P.op("act", lambda e: e.activation(out=lnt, in_=psms, func=AF.Ln, scale=1.0 / n, bias=EPS),
                     r=[("ps", pb)], w=[(rstdtag, "ln")])
                P.op("act", lambda e: e.activation(out=rstd, in_=lnt, func=AF.Exp, scale=-0.5),
                     r=[(rstdtag, "ln")], w=[rstdtag])

            if kind == 0:
                Wb = A.get(8 * 3072)
                stg = [A.get(3072, F32), A.get(3072, F32)]
                gqk = A.get(2, F32)
                P.op("sp", lambda e: [e.dma_start(out=gqk[:, 0:1], in_=gq[:, :]),
                                      e.dma_start(out=gqk[:, 1:2], in_=gk[:, :])],
                     w=["gains"], semkey="gains", ndma=2)
                load_cast_weight(Wb, "Wb", w_in, 8, 3072, stg, "stg")
                Wv = Wb.rearrange("p (c n) -> p c n", c=8)
                xts = [A.get(4096, F32), A.get(4096, F32)]
                hb = A.get(4096)
                hT = [A.get(512) for _ in range(8)]
                st4 = A.get(16, F32)
                tabs = [(A.get(512, F32), A.get(512, F32)) for _ in range(2)]
                sq = A.get(512)
                tmp = (A.get(512), A.get(512, F32), A.get(512, F32))
                lnt = A.get(512, F32)
                rstd = A.get(512, F32)
                kf = [A.get(512), A.get(512)]
                vb = A.get(4096)

                def proj_phase(src, ntiles, cos_d, sin_d, do_kv, toff):
                    for t in range(ntiles):
                        xt = xts[(t + toff) % 2]
                        xtag = ("x", (t + toff) % 2)
                        load_x_tile(src, t, xt, xtag, gathered=(do_kv and xb_gathered))
                        tb = tabs[(t + toff) % 2]
                        ttag = ("tab", (t + toff) % 2)
                        P.op("sp", lambda e, t=t, tb=tb: [
                            e.dma_start(out=tb[0], in_=cos_d[:, t * 512:(t + 1) * 512]),
                            e.dma_start(out=tb[1], in_=sin_d[:, t * 512:(t + 1) * 512])],
                            w=[ttag], semkey=ttag, ndma=2)
                        norm_tile(xt, xtag, hb, "hb", st4, "st4")
                        transpose_tile_bf(hb, "hb", hT, "hT", gat, (0, 1))
                        heads = range(NH)
                        for h in heads:
                            col0 = (D + h * 128) if do_kv else (h * 128)
                            pk = 2 + (h % 2)
                            pkt = bank(pk)
                            P.op("pe", lambda e, col0=col0, pkt=pkt: [
                                e.matmul(pkt, lhsT=Wv[:, c, col0:col0 + 128], rhs=hT[c],
                                         start=(c == 0), stop=(c == 7)) for c in range(8)],
                                r=[("Wb", c) for c in range(8)] + [("hT", c) for c in range(8)],
                                w=[("ps", pk)])
                            P.op("act", lambda e, pkt=pkt: e.activation(out=sq, in_=pkt, func=AF.Square),
                                 r=[("ps", pk)], w=["sq"])
                            P.op("pe", lambda e: e.matmul(bank(4), lhsT=onesbd, rhs=sq, start=True, stop=True),
                                 r=["sq", "const"], w=[("ps", 4)])
                            rstd_from_ps(bank(4), 4, rstd, "rstd", lnt, 64)
                            kfb = kf[h % 2]
                            kftag = ("kf", h % 2)
                            feature_head_post(pkt, pk, 128, gqk[:, (1 if do_kv else 0):(2 if do_kv else 1)],
                                              tb[0], tb[1], ttag, kfb, kftag, rstd, "rstd", tmp, "tmp", 5)
                            dst = KT if do_kv else QT
                            P.op("pool", lambda e, h=h, t=t, kfb=kfb, dst=dst: e.dma_start(
                                out=dst[h, :, t * 512:(t + 1) * 512], in_=kfb),
                                r=[kftag], semkey=("kst", h % 2))
                        if do_kv:
                            for j in range(4):
                                for half in range(2):
                                    pv = 6 + (half % 2)
                                    P.op("pe", lambda e, j=j, half=half, pv=pv: [
                                        e.matmul(bank(pv), lhsT=hT[c][:, j * 128:(j + 1) * 128],
                                                 rhs=Wv[:, c, 2 * D + half * 512:2 * D + (half + 1) * 512],
                                                 start=(c == 0), stop=(c == 7)) for c in range(8)],
                                        r=[("Wb", c) for c in range(8)] + [("hT", c) for c in range(8)],
                                        w=[("ps", pv)])
                                    if half == 0:
                                        P.op("dve", lambda e, j=j, half=half, pv=pv: e.tensor_copy(
                                            out=vb[:, j * 1024 + half * 512:j * 1024 + (half + 1) * 512],
                                            in_=bank(pv)), r=[("ps", pv)], w=[("vb", j, half)])
                                    else:
                                        P.op("act", lambda e, j=j, half=half, pv=pv: e.activation(
                                            out=vb[:, j * 1024 + half * 512:j * 1024 + (half + 1) * 512],
                                            in_=bank(pv), func=AF.Copy), r=[("ps", pv)], w=[("vb", j, half)])
                                P.op("pool", lambda e, j=j, t=t: e.dma_start(
                                    out=VS[:, :, t * 4 + j, :].rearrange("h p d -> p h d"),
                                    in_=vb[:, j * 1024:(j + 1) * 1024].rearrange("p (h d) -> p h d", h=NH)),
                                    r=[("vb", j, 0), ("vb", j, 1)], semkey=("vst", j))

                proj_phase(xb, NT, cosk, sink, True, 0)
                proj_phase(xo, NTO, cosq, sinq, False, NT)
            else:
                Wa = A.get(8 * 704)
                Wav = Wa.rearrange("p (c n) -> p c n", c=8)
                Wk = A.get(2 * 1024)
                Wkv_ = Wk.rearrange("p (f n) -> p f n", f=2)
                Wvv = A.get(2 * 1024)
                Wvv_ = Wvv.rearrange("p (f n) -> p f n", f=2)
                Wqb = A.get(3 * 1536)
                Wqv = Wqb.rearrange("p (f n) -> p f n", f=3)
                stg = [A.get(3072, F32), A.get(3072, F32)]
                gt = A.get(16, F32)
                P.op("sp", lambda e: [e.dma_start(out=gt[:, 0:3], in_=g_qa[:, :]),
                                      e.dma_start(out=gt[:, 3:5], in_=g_kva[:, :]),
                                      e.dma_start(out=gt[:, 5:6], in_=gqn[:, :]),
                                      e.dma_start(out=gt[0:64, 6:7], in_=gqr[:, :]),
                                      e.dma_start(out=gt[:, 7:8], in_=gkn[:, :]),
                                      e.dma_start(out=gt[0:64, 8:9], in_=gkr[:, :])],
                     w=["gains"], semkey="gains", ndma=6)
                load_cast_weight(Wa, "Wa", w_a, 8, 704, stg, "stg")
                load_cast_weight(Wqb, "Wqb", w_qb, 3, 1536, stg, "stg")
                for f in range(2):
                    s_ = stg[f % 2]
                    stag = ("stg", f % 2)
                    P.op("sp", lambda e, f=f, s_=s_: e.dma_start(out=s_[:, 0:2048], in_=w_kvb[f * 128:(f + 1) * 128, :]),
                         w=[stag], semkey=stag)
                    sv_ = s_[:, 0:2048].rearrange("p (h t d) -> p h t d", h=8, t=2)
                    P.op("pool", lambda e, f=f, sv_=sv_: e.tensor_copy(
                        out=Wk[:, f * 1024:(f + 1) * 1024].rearrange("p (h d) -> p h d", h=8), in_=sv_[:, :, 0, :]),
                        r=[stag], w=[("Wk", f)])
                    P.op("dve", lambda e, f=f, sv_=sv_: e.tensor_copy(
                        out=Wvv[:, f * 1024:(f + 1) * 1024].rearrange("p (h d) -> p h d", h=8), in_=sv_[:, :, 1, :]),
                        r=[stag], w=[("Wv", f)])
                xts = [A.get(4096, F32), A.get(4096, F32)]
                hb = A.get(4096)
                hT = [A.get(512) for _ in range(8)]
                st4 = A.get(16, F32)
                tabs = [(A.get(512, F32), A.get(512, F32)) for _ in range(2)]
                sqc = [A.get(512) for _ in range(3)]
                cn = [A.get(512) for _ in range(3)]
                sq = A.get(512)
                sqr = A.get(512)
                tmp = (A.get(512), A.get(512, F32), A.get(512, F32))
                lnt = A.get(512, F32)
                rstd = A.get(512, F32)
                rstdc = A.get(512, F32)
                rp = A.get(512, F32)
                kf = [A.get(512), A.get(512)]
                krf = [A.get(512), A.get(512)]
                vb = A.get(4096)

                def mla_phase(src, ntiles, cos_d, sin_d, do_kv, toff):
                    for t in range(ntiles):
                        xt = xts[(t + toff) % 2]
                        xtag = ("x", (t + toff) % 2)
                        load_x_tile(src, t, xt, xtag, gathered=(do_kv and xb_gathered))
                        tb = tabs[(t + toff) % 2]
                        ttag = ("tab", (t + toff) % 2)
                        P.op("sp", lambda e, t=t, tb=tb: [
                            e.dma_start(out=tb[0], in_=cos_d[:, t * 512:(t + 1) * 512]),
                            e.dma_start(out=tb[1], in_=sin_d[:, t * 512:(t + 1) * 512])],
                            w=[ttag], semkey=ttag, ndma=2)
                        norm_tile(xt, xtag, hb, "hb", st4, "st4")
                        transpose_tile_bf(hb, "hb", hT, "hT", gat, (0, 1))
                        hTr = [("hT", c) for c in range(8)]
                        War = [("Wa", c) for c in range(8)]
                        nf = 2 if do_kv else 3
                        col0 = 384 if do_kv else 0
                        gcol0 = 3 if do_kv else 0
                        cbanks = (2, 3, 5)
                        for f in range(nf):
                            pb = cbanks[f]
                            P.op("pe", lambda e, f=f, pb=pb: [
                                e.matmul(bank(pb), lhsT=Wav[:, c, col0 + f * 128:col0 + (f + 1) * 128], rhs=hT[c],
                                         start=(c == 0), stop=(c == 7)) for c in range(8)],
                                r=War + hTr, w=[("ps", pb)])
                            P.op("act", lambda e, f=f, pb=pb: e.activation(out=sqc[f], in_=bank(pb), func=AF.Square),
                                 r=[("ps", pb)], w=[("sqc", f)])
                        P.op("pe", lambda e: [e.matmul(bank(4), lhsT=ones, rhs=sqc[f], start=(f == 0), stop=(f == nf - 1))
                                              for f in range(nf)],
                             r=[("sqc", f) for f in range(nf)] + ["const"], w=[("ps", 4)])
                        rstd_from_ps(bank(4), 4, rstdc, "rstdc", lnt, 128 * nf)
                        for f in range(nf):
                            pb = cbanks[f]
                            P.op("dve", lambda e, f=f, pb=pb: e.scalar_tensor_tensor(
                                out=cn[f], in0=bank(pb), scalar=gt[:, gcol0 + f:gcol0 + f + 1], in1=rstdc,
                                op0=ALU.mult, op1=ALU.mult),
                                r=[("ps", pb), "gains", "rstdc"], w=[("cn", f)])
                        cnr = [("cn", f) for f in range(nf)]
                        if do_kv:
                            P.op("pe", lambda e: [
                                e.matmul(bank(5)[0:64, :], lhsT=Wav[:, c, 640:704], rhs=hT[c],
                                         start=(c == 0), stop=(c == 7)) for c in range(8)],
                                r=War + hTr, w=[("ps", 5)])
                            P.op("act", lambda e: e.activation(out=sqr[0:64, :], in_=bank(5)[0:64, :], func=AF.Square),
                                 r=[("ps", 5)], w=["sqr"])
                            feature_head_post(bank(5)[0:64, :], 5, 64, gt[0:64, 8:9], tb[0], tb[1], ttag,
                                              rp[0:64, :], "rp", None, None, tmp, "tmp", 6)
                        for h in range(NH):
                            pk = 2 + (h % 2)
                            if do_kv:
                                P.op("pe", lambda e, h=h, pk=pk: [
                                    e.matmul(bank(pk), lhsT=Wkv_[:, f, h * 128:(h + 1) * 128], rhs=cn[f],
                                             start=(f == 0), stop=(f == 1)) for f in range(2)],
                                    r=cnr + [("Wk", 0), ("Wk", 1)], w=[("ps", pk)])
                            else:
                                P.op("pe", lambda e, h=h, pk=pk: [
                                    e.matmul(bank(pk), lhsT=Wqv[:, f, h * 192:h * 192 + 128], rhs=cn[f],
                                             start=(f == 0), stop=(f == 2)) for f in range(3)],
                                    r=cnr + [("Wqb", f) for f in range(3)], w=[("ps", pk)])
                                P.op("pe", lambda e, h=h: [
                                    e.matmul(bank(5)[0:64, :], lhsT=Wqv[:, f, h * 192 + 128:h * 192 + 192], rhs=cn[f],
                                             start=(f == 0), stop=(f == 2)) for f in range(3)],
                                    r=cnr + [("Wqb", f) for f in range(3)], w=[("ps", 5)])
                                P.op("act", lambda e: e.activation(out=sqr[0:64, :], in_=bank(5)[0:64, :], func=AF.Square),
                                     r=[("ps", 5)], w=["sqr"])
                            P.op("act", lambda e, pk=pk: e.activation(out=sq, in_=bank(pk), func=AF.Square),
                                 r=[("ps", pk)], w=["sq"])
                            P.op("pe", lambda e: [
                                e.matmul(bank(4), lhsT=ones, rhs=sq, start=True, stop=False),
                                e.matmul(bank(4), lhsT=ones[0:64, :], rhs=sqr[0:64, :], start=False, stop=True)],
                                r=["sq", "sqr", "const"], w=[("ps", 4)])
                            rstd_from_ps(bank(4), 4, rstd, "rstd", lnt, 192)
                            kfb = kf[h % 2]
                            kftag = ("kf", h % 2)
                            gc = 7 if do_kv else 5
                            P.op("dve", lambda e, pk=pk, kfb=kfb, gc=gc: e.scalar_tensor_tensor(
                                out=kfb, in0=bank(pk), scalar=gt[:, gc:gc + 1], in1=rstd, op0=ALU.mult, op1=ALU.mult),
                                r=[("ps", pk), "gains", "rstd"], w=[kftag])
                            dstn = KT if do_kv else QT
                            P.op("pool", lambda e, h=h, t=t, kfb=kfb, dstn=dstn: e.dma_start(
                                out=dstn[h, :, t * 512:(t + 1) * 512], in_=kfb),
                                r=[kftag], semkey=("kst", h % 2))
                            krb = krf[h % 2]
                            krtag = ("krf", h % 2)
                            if do_kv:
                                P.op("pool", lambda e, krb=krb: e.tensor_tensor(
                                    out=krb[0:64, :], in0=rp[0:64, :], in1=rstd[0:64, :], op=ALU.mult),
                                    r=["rp", "rstd"], w=[krtag])
                            else:
                                feature_head_post(bank(5)[0:64, :], 5, 64, gt[0:64, 6:7], tb[0], tb[1], ttag,
                                                  krb[0:64, :], krtag, rstd, "rstd", tmp, "tmp", 6)
                            dstr = KR if do_kv else QR
                            P.op("pool", lambda e, h=h, t=t, krb=krb, dstr=dstr: e.dma_start(
                                out=dstr[h, :, t * 512:(t + 1) * 512], in_=krb[0:64, :]),
                                r=[krtag], semkey=("krst", h % 2))
                        if do_kv:
                            for j in range(4):
                                for half in range(2):
                                    pv = 7
                                    P.op("pe", lambda e, j=j, half=half, pv=pv: [
                                        e.matmul(bank(pv), lhsT=cn[f][:, j * 128:(j + 1) * 128],
                                                 rhs=Wvv_[:, f, half * 512:(half + 1) * 512],
                                                 start=(f == 0), stop=(f == 1)) for f in range(2)],
                                        r=cnr + [("Wv", 0), ("Wv", 1)], w=[("ps", pv)])
                                    P.op("act", lambda e, j=j, half=half, pv=pv: e.activation(
                                        out=vb[:, j * 1024 + half * 512:j * 1024 + (half + 1) * 512],
                                        in_=bank(pv), func=AF.Copy), r=[("ps", pv)], w=[("vb", j, half)])
                                P.op("pool", lambda e, j=j, t=t: e.dma_start(
                                    out=VS[:, :, t * 4 + j, :].rearrange("h p d -> p h d"),
                                    in_=vb[:, j * 1024:(j + 1) * 1024].rearrange("p (h d) -> p h d", h=NH)),
                                    r=[("vb", j, 0), ("vb", j, 1)], semkey=("vst", j))

                mla_phase(xb, NT, cosk, sink, True, 0)
                mla_phase(xo, NTO, cosq, sinq, False, NT)

            P.barrier()
            A.reset()

            NCH = NKB // 16
            Kt = A.get(S)
            Vt = A.get(NKB * 130)
            Vv = Vt.rearrange("p (k d) -> p k d", d=130)
            Qt = A.get(SO)
            if kind == 1:
                Kr = A.get(S)
                Qr = A.get(SO)
            msk = A.get(16 * 512)
            Pt = [A.get(1024), A.get(1024)]
            o1 = A.get(128, F32)
            oo = A.get(128, F32)
            onb = A.get(128)
            attT = A.get(512)
            sg = A.get(128, F32)
            lamt = A.get(4 * 64, F32)
            cst = A.get(16, F32)
            P.op("sp", lambda e: e.dma_start(out=msk, in_=maskd[:, :]), w=["msk"], semkey="msk")
            P.op("pool", lambda e: e.memset(Vv[:, :, 128:130], 1.0), w=[("V", c) for c in range(NCH)])
            if kind == 0:
                lam_init = 0.8 - 0.6 * math.exp(-0.3 * 0)
                P.op("sp", lambda e: [e.dma_start(out=sg, in_=subln.partition_broadcast(128)),
                                      e.dma_start(out=lamt, in_=lam4.partition_broadcast(128))],
                     w=["sg", "lamt"], semkey="cC", ndma=2)
                P.op("dve", lambda e: e.tensor_scalar(out=sg, in0=sg, scalar1=1.0 - lam_init, scalar2=None,
                                                      op0=ALU.mult), r=["sg"], w=["sg"])
                P.op("dve", lambda e: e.tensor_tensor(out=lamt[:, 0:64], in0=lamt[:, 0:64], in1=lamt[:, 64:128],
                                                      op=ALU.mult), r=["lamt"], w=["lam_a"])
                P.op("dve", lambda e: e.tensor_tensor(out=lamt[:, 128:192], in0=lamt[:, 128:192],
                                                      in1=lamt[:, 192:256], op=ALU.mult), r=["lamt"], w=["lam_b"])
                P.op("dve", lambda e: e.reduce_sum(out=cst[:, 0:1], in_=lamt[:, 0:64], axis=AX.X),
                     r=["lam_a"], w=["lam_s1"])
                P.op("dve", lambda e: e.reduce_sum(out=cst[:, 1:2], in_=lamt[:, 128:192], axis=AX.X),
                     r=["lam_b"], w=["lam_s2"])
                P.op("act", lambda e: e.activation(out=cst[:, 2:4], in_=cst[:, 0:2], func=AF.Exp),
                     r=["lam_s1", "lam_s2"], w=["lam_e"])
                P.op("dve", lambda e: e.tensor_tensor(out=cst[:, 4:5], in0=cst[:, 3:4], in1=cst[:, 2:3],
                                                      op=ALU.subtract), r=["lam_e"], w=["lam_d"])
                P.op("dve", lambda e: e.tensor_scalar(out=cst[:, 5:6], in0=cst[:, 4:5], scalar1=-lam_init,
                                                      scalar2=None, op0=ALU.add), r=["lam_d"], w=["neglam"])
                neglam = cst[:, 5:6]
                NSM = 2
                scale = 64 ** -0.5
            else:
                NSM = 1
                scale = 192 ** -0.5

            def oslot(s):
                return ps[:, (4 + s // 3) * 512 + (s % 3) * 129:(4 + s // 3) * 512 + (s % 3) * 129 + 129]

            def otag(s):
                return ("psO", 4 + s // 3)

            Osb = A.get(8 * 129, F32)

            def osb(s):
                return Osb[:, s * 129:(s + 1) * 129]

            def osbtag(s):
                return ("Osb", s // 3)

            step = 0
            for h in range(NH):
                for c in range(NCH):
                    if kind == 0:
                        P.op("sp", lambda e, h=h, c=c: e.dma_start(
                            out=Kt[:, c * 2048:(c + 1) * 2048], in_=KT[h, :, c * 2048:(c + 1) * 2048]),
                            w=[("K", c)], semkey=("K", c))
                    else:
                        P.op("sp", lambda e, h=h, c=c: [
                            e.dma_start(out=Kt[:, c * 2048:(c + 1) * 2048], in_=KT[h, :, c * 2048:(c + 1) * 2048]),
                            e.dma_start(out=Kr[0:64, c * 2048:(c + 1) * 2048], in_=KR[h, :, c * 2048:(c + 1) * 2048])],
                            w=[("K", c)], semkey=("K", c), ndma=2)
                    P.op("sp", lambda e, h=h, c=c: e.dma_start(
                        out=Vv[:, c * 16:(c + 1) * 16, 0:128], in_=VS[h, :, c * 16:(c + 1) * 16, :]),
                        w=[("V", c)], semkey=("V", c))
                if kind == 0:
                    P.op("sp", lambda e, h=h: e.dma_start(out=Qt, in_=QT[h, :, :]), w=["Q"], semkey="Q")
                else:
                    P.op("sp", lambda e, h=h: [e.dma_start(out=Qt, in_=QT[h, :, :]),
                                               e.dma_start(out=Qr[0:64, :], in_=QR[h, :, :])],
                         w=["Q"], semkey="Q", ndma=2)
                for g in range(NG):
                    nkb = 16 * g + 16
                    for kb in range(nkb):
                        jb = kb - 16 * g
                        j0 = 0 if jb < 0 else jb // 4
                        q0 = j0 * 128
                        nq = 512 - q0
                        b = step % 2
                        step += 1
                        pS = ps[:, b * 1024:(b + 1) * 1024]
                        c = kb // 16
                        if kind == 0:
                            P.op("pe", lambda e, pS=pS, kb=kb, g=g, q0=q0, nq=nq: [
                                e.matmul(pS[:, q0:512], lhsT=Kt[0:64, kb * 128:(kb + 1) * 128],
                                         rhs=Qt[0:64, g * 512 + q0:(g + 1) * 512], start=True, stop=True),
                                e.matmul(pS[:, 512 + q0:1024], lhsT=Kt[64:128, kb * 128:(kb + 1) * 128],
                                         rhs=Qt[64:128, g * 512 + q0:(g + 1) * 512], start=True, stop=True)],
                                r=[("K", c), "Q"], w=[("psS", b)])
                        else:
                            P.op("pe", lambda e, pS=pS, kb=kb, g=g, q0=q0, nq=nq: [
                                e.matmul(pS[:, q0:512], lhsT=Kt[:, kb * 128:(kb + 1) * 128],
                                         rhs=Qt[:, g * 512 + q0:(g + 1) * 512], start=True, stop=False),
                                e.matmul(pS[:, q0:512], lhsT=Kr[0:64, kb * 128:(kb + 1) * 128],
                                         rhs=Qr[0:64, g * 512 + q0:(g + 1) * 512], start=False, stop=True)],
                                r=[("K", c), "Q"], w=[("psS", b)])
                        pt = Pt[b]
                        if q0 == 0:
                            P.op("act", lambda e, pS=pS, pt=pt: e.activation(
                                out=pt[:, 0:512 * NSM], in_=pS[:, 0:512 * NSM], func=AF.Exp, scale=scale),
                                r=[("psS", b)], w=[("Pt", b)])
                        else:
                            P.op("act", lambda e, pS=pS, pt=pt, q0=q0: [
                                e.activation(out=pt[:, m * 512 + q0:(m + 1) * 512],
                                             in_=pS[:, m * 512 + q0:(m + 1) * 512], func=AF.Exp, scale=scale)
                                for m in range(NSM)],
                                r=[("psS", b)], w=[("Pt", b)])
                        if jb >= 0:
                            P.op("dve", lambda e, pt=pt, jb=jb, q0=q0: [
                                e.tensor_tensor(out=pt[:, m * 512 + q0:(m + 1) * 512],
                                                in0=pt[:, m * 512 + q0:(m + 1) * 512],
                                                in1=msk[:, jb * 512 + q0:(jb + 1) * 512], op=ALU.mult)
                                for m in range(NSM)],
                                r=[("Pt", b), "msk"], w=[("Pt", b)])
                        mm = []
                        wl = []
                        started = set()
                        for j in range(j0, 4):
                            last = 16 * g + 4 * j + 3
                            for m in range(NSM):
                                s = m * 4 + j
                                bk = 4 + s // 3
                                st_ = (kb == 0) and (bk not in started)
                                started.add(bk)
                                slots_in_bank = [s2 for s2 in range(4 * NSM) if 4 + s2 // 3 == bk]
                                bank_last = max(16 * g + 4 * (s2 % 4) + 3 for s2 in slots_in_bank)
                                s_last = max(s2 for s2 in slots_in_bank if 16 * g + 4 * (s2 % 4) + 3 == bank_last)
                                sp_ = (kb == bank_last) and (s == s_last)
                                mm.append((s, m * 512 + j * 128, st_, sp_))
                                wl.append(otag(s))
                        P.op("pe", lambda e, mm=mm, pt=pt, kb=kb: [
                            e.matmul(oslot(s), lhsT=pt[:, o_:o_ + 128], rhs=Vv[:, kb, 0:129],
                                     start=st_, stop=sp_) for (s, o_, st_, sp_) in mm],
                            r=[("Pt", b), ("V", c)], w=wl)
                    pT = bank_bf(7)[:, 0:512]
                    nslots = 4 * NSM
                    for bi in range((nslots + 2) // 3):
                        ns_ = min(3, nslots - bi * 3)
                        P.op("dve", lambda e, bi=bi, ns_=ns_: e.tensor_copy(
                            out=Osb[:, bi * 387:bi * 387 + ns_ * 129],
                            in_=ps[:, (4 + bi) * 512:(4 + bi) * 512 + ns_ * 129]),
                            r=[("psO", 4 + bi)], w=[("Osb", bi)])
                    for j in range(4):
                        O1 = osb(j)
                        if kind == 0:
                            O2 = osb(4 + j)
                            P.op("dve", lambda e, O1=O1: e.reciprocal(out=cst[:, 8:9], in_=O1[:, 128:129]),
                                 r=[osbtag(j)], w=["rl1"])
                            P.op("dve", lambda e, O2=O2: e.reciprocal(out=cst[:, 9:10], in_=O2[:, 128:129]),
                                 r=[osbtag(4 + j)], w=["rl2"])
                            P.op("dve", lambda e: e.tensor_tensor(out=cst[:, 10:11], in0=cst[:, 9:10], in1=neglam,
                                                                  op=ALU.mult), r=["rl2", "neglam"], w=["rl2m"])
                            P.op("dve", lambda e, O1=O1: e.tensor_scalar(out=o1, in0=O1[:, 0:128], scalar1=cst[:, 8:9],
                                                                         scalar2=None, op0=ALU.mult),
                                 r=[osbtag(j), "rl1"], w=["o1"])
                            P.op("dve", lambda e, O2=O2: e.scalar_tensor_tensor(
                                out=oo, in0=O2[:, 0:128], scalar=cst[:, 10:11], in1=o1, op0=ALU.mult, op1=ALU.add),
                                r=[osbtag(4 + j), "rl2m", "o1"], w=["oo"])
                            P.op("act", lambda e: e.activation(out=junk[:, 0:128], in_=oo, func=AF.Square,
                                                               accum_out=cst[:, 11:12]),
                                 r=["oo"], w=["junk", "oss"])
                            P.op("act", lambda e: e.activation(out=cst[:, 12:13], in_=cst[:, 11:12], func=AF.Ln,
                                                               scale=1.0 / 128, bias=EPS), r=["oss"], w=["oln"])
                            P.op("act", lambda e: e.activation(out=cst[:, 13:14], in_=cst[:, 12:13], func=AF.Exp,
                                                               scale=-0.5), r=["oln"], w=["ors"])
                            P.op("dve", lambda e: e.scalar_tensor_tensor(
                                out=onb, in0=oo, scalar=cst[:, 13:14], in1=sg, op0=ALU.mult, op1=ALU.mult),
                                r=["oo", "ors", "sg"], w=["onb"])
                        else:
                            P.op("dve", lambda e, O1=O1: e.reciprocal(out=cst[:, 8:9], in_=O1[:, 128:129]),
                                 r=[osbtag(j)], w=["rl1"])
                            P.op("dve", lambda e, O1=O1: e.tensor_scalar(out=onb, in0=O1[:, 0:128], scalar1=cst[:, 8:9],
                                                                         scalar2=None, op0=ALU.mult),
                                 r=[osbtag(j), "rl1"], w=["onb"])
                        P.op("pe", lambda e, j=j: e.transpose(out=pT[:, j * 128:(j + 1) * 128], in_=onb,
                                                              identity=identb),
                             r=["onb", "const"], w=[("ps", 7)])
                    P.op("act", lambda e: e.activation(out=attT, in_=pT, func=AF.Copy), r=[("ps", 7)], w=["attT"])
                    P.op("pool", lambda e, h=h, g=g: e.dma_start(out=AT[h, :, g * 512:(g + 1) * 512], in_=attT),
                         r=["attT"], semkey="ast")

            P.barrier()
            A.reset()

            NSB = ST // 128
            NTS = ST // 512
            acc = A.get(NSB * 1024, F32)
            h2T = [A.get(ST) for _ in range(8)]
            comb = A.get(NSB * 64, F32)
            Wo = A.get(8 * 1024)
            Wov = Wo.rearrange("p (h n) -> p h n", h=8)
            wrt = A.get(8 * 72, F32)
            wrv = wrt.rearrange("p (c n) -> p c n", c=8)
            brt = A.get(72, F32)
            stgD = [A.get(2048, F32) for _ in range(3)]
            Wg = [A.get(2048), A.get(2048)]
            Wu = [A.get(2048), A.get(2048)]
            Wd = [A.get(2048), A.get(2048)]
            attl = A.get(8 * 512)
            attv = attl.rearrange("p (h n) -> p h n", h=8)
            xot = A.get(4096, F32)
            h2 = xot
            h2Tf = [A.get(512, F32) for _ in range(8)]
            st4 = A.get(16, F32)
            lg = A.get(72, F32)
            rt = A.get(8 * 64 // 2, F32)
            sgt = [A.get(512, F32), A.get(512, F32)]
            hid = [[A.get(512), A.get(512)], [A.get(512), A.get(512)]]
            cr = A.get(16, F32)

            load_cast_weight(Wo, "Wo", w_out, 8, 1024, stgD[0:2], "stgD")
            P.op("sp", lambda e: [e.dma_start(out=wrv, in_=wr.rearrange("(c p) n -> p c n", p=128)),
                                  e.dma_start(out=brt, in_=br.partition_broadcast(128))],
                 w=["wr"], semkey="wr", ndma=2)

            def router_block(psl, pb, blk):
                gl = lg[:, 0:8]
                el = lg[:, 8:72]
                R = lambda a, b_: rt[:, a:b_]
                ml, oh1, ml2, oh2, pen, ohg, eg = R(0, 64), R(64, 128), R(128, 192), R(192, 256), cr[:, 8:16], None, small[:, 0:8]
                P.op("dve", lambda e: e.tensor_tensor(out=lg, in0=psl, in1=brt, op=ALU.add),
                     r=[("ps", pb), "wr"], w=["lg"])
                P.op("dve", lambda e: e.reduce_max(out=cr[:, 0:1], in_=gl, axis=AX.X), r=["lg"], w=["gmax"])
                P.op("dve", lambda e: e.tensor_scalar(out=cr[:, 1:2], in0=cr[:, 0:1], scalar1=-1.0, scalar2=None,
                                                      op0=ALU.mult), r=["gmax"], w=["ngmax"])
                P.op("act", lambda e: e.activation(out=eg, in_=gl, func=AF.Exp, bias=cr[:, 1:2], scale=1.0,
                                                   accum_out=cr[:, 2:3]),
                     r=["lg", "ngmax"], w=["eg", "gsum"])
                P.op("dve", lambda e: e.reciprocal(out=cr[:, 3:4], in_=cr[:, 2:3]), r=["gsum"], w=["gw"])
                P.op("dve", lambda e: e.tensor_scalar(out=pen, in0=gl, scalar1=cr[:, 0:1], scalar2=None,
                                                      op0=ALU.is_ge), r=["lg", "gmax"], w=["pen0"])
                P.op("dve", lambda e: e.tensor_scalar(out=pen, in0=pen, scalar1=-1.0, scalar2=1e30,
                                                      op0=ALU.add, op1=ALU.mult), r=["pen0"], w=["pen"])
                for gi in range(8):
                    P.op("dve", lambda e, gi=gi: e.tensor_scalar(
                        out=ml[:, gi * 8:(gi + 1) * 8], in0=el[:, gi * 8:(gi + 1) * 8],
                        scalar1=pen[:, gi:gi + 1], scalar2=None, op0=ALU.add),
                        r=["lg", "pen"], w=[("ml", gi)])
                mlr = [("ml", gi) for gi in range(8)]
                P.op("dve", lambda e: e.reduce_max(out=cr[:, 4:5], in_=ml, axis=AX.X), r=mlr, w=["m1"])
                P.op("dve", lambda e: e.tensor_scalar(out=oh1, in0=ml, scalar1=cr[:, 4:5], scalar2=None,
                                                      op0=ALU.is_ge), r=mlr + ["m1"], w=["oh1"])
                P.op("dve", lambda e: e.scalar_tensor_tensor(out=ml2, in0=oh1, scalar=-1e30, in1=ml,
                                                             op0=ALU.mult, op1=ALU.add),
                     r=mlr + ["oh1"], w=["ml2"])
                P.op("dve", lambda e: e.reduce_max(out=cr[:, 5:6], in_=ml2, axis=AX.X), r=["ml2"], w=["m2"])
                P.op("dve", lambda e: e.tensor_scalar(out=oh2, in0=ml2, scalar1=cr[:, 5:6], scalar2=None,
                                                      op0=ALU.is_ge), r=["ml2", "m2"], w=["oh2"])
                P.op("dve", lambda e: e.tensor_tensor(out=cr[:, 6:7], in0=cr[:, 5:6], in1=cr[:, 4:5],
                                                      op=ALU.subtract), r=["m1", "m2"], w=["dd"])
                P.op("act", lambda e: e.activation(out=cr[:, 7:8], in_=cr[:, 6:7], func=AF.Exp), r=["dd"], w=["ed"])
                P.op("dve", lambda e: e.tensor_scalar(out=cr[:, 6:7], in0=cr[:, 7:8], scalar1=1.0, scalar2=None,
                                                      op0=ALU.add), r=["ed"], w=["den"])
                P.op("dve", lambda e: e.reciprocal(out=cr[:, 7:8], in_=cr[:, 6:7]), r=["den"], w=["rden"])
                P.op("dve", lambda e: e.tensor_tensor(out=cr[:, 6:7], in0=cr[:, 7:8], in1=cr[:, 3:4], op=ALU.mult),
                     r=["rden", "gw"], w=["w1"])
                P.op("dve", lambda e: e.tensor_tensor(out=cr[:, 7:8], in0=cr[:, 3:4], in1=cr[:, 6:7],
                                                      op=ALU.subtract), r=["w1", "gw"], w=["w2"])
                cb = comb[:, blk * 64:(blk + 1) * 64]
                P.op("dve", lambda e: e.tensor_scalar(out=cb, in0=oh1, scalar1=cr[:, 6:7], scalar2=None,
                                                      op0=ALU.mult), r=["oh1", "w1"], w=[("comb", blk)])
                P.op("dve", lambda e: e.scalar_tensor_tensor(out=cb, in0=oh2, scalar=cr[:, 7:8], in1=cb,
                                                             op0=ALU.mult, op1=ALU.add),
                     r=["oh2", "w2", ("comb", blk)], w=[("comb", blk)])

            for sti in range(SO // ST):
                for tt in range(NTS):
                    t = sti * NTS + tt
                    P.op("sp", lambda e, t=t: e.dma_start(
                        out=attv, in_=AT[:, :, t * 512:(t + 1) * 512].rearrange("h p n -> p h n")),
                        w=["attl"], semkey="attl")
                    load_x_tile(xo, t, xot, "xot", extra_w=[("h2", j_) for j_ in range(4)])
                    for j in range(4):
                        blk = tt * 4 + j
                        for half in range(2):
                            pb = half
                            P.op("pe", lambda e, j=j, half=half, pb=pb: [
                                e.matmul(bank(pb), lhsT=attv[:, hh, j * 128:(j + 1) * 128],
                                         rhs=Wov[:, hh, half * 512:(half + 1) * 512],
                                         start=(hh == 0), stop=(hh == 7)) for hh in range(8)],
                                r=["attl"] + [("Wo", c) for c in range(8)], w=[("ps", pb)])
                            P.op("dve", lambda e, j=j, half=half, pb=pb, blk=blk: e.tensor_tensor(
                                out=acc[:, blk * 1024 + half * 512:blk * 1024 + (half + 1) * 512],
                                in0=bank(pb), in1=xot[:, j * 1024 + half * 512:j * 1024 + (half + 1) * 512],
                                op=ALU.add), r=[("ps", pb), "xot"], w=[("acc", blk, half)])
                        P.op("act", lambda e, j=j, blk=blk: e.activation(
                            out=junk, in_=acc[:, blk * 1024:(blk + 1) * 1024], func=AF.Square,
                            accum_out=st4[:, j:j + 1]),
                            r=[("acc", blk, 0), ("acc", blk, 1)], w=["junk", ("st4", j)])
                    P.op("act", lambda e: e.activation(out=st4[:, 4:8], in_=st4[:, 0:4], func=AF.Ln,
                                                       scale=1.0 / D, bias=EPS),
                         r=[("st4", j) for j in range(4)], w=[("st4", "ln")])
                    P.op("act", lambda e: e.activation(out=st4[:, 8:12], in_=st4[:, 4:8], func=AF.Exp, scale=-0.5),
                         r=[("st4", "ln")], w=[("st4", "rs")])
                    for j in range(4):
                        blk = tt * 4 + j
                        P.op("act", lambda e, j=j, blk=blk: e.activation(
                            out=h2[:, j * 1024:(j + 1) * 1024], in_=acc[:, blk * 1024:(blk + 1) * 1024],
                            func=AF.Copy, scale=st4[:, 8 + j:9 + j]),
                            r=[("acc", blk, 0), ("acc", blk, 1), ("st4", "rs")], w=[("h2", j)])
                    for c in range(8):
                        pb = 2 + c % 2
                        P.op("pe", lambda e, c=c, pb=pb: [
                            e.transpose(out=bank(pb)[:, j * 128:(j + 1) * 128],
                                        in_=h2[:, j * 1024 + c * 128:j * 1024 + (c + 1) * 128], identity=identf)
                            for j in range(4)],
                            r=[("h2", j) for j in range(4)] + ["const"], w=[("ps", pb)])
                        P.op("act", lambda e, c=c, pb=pb: e.activation(
                            out=h2Tf[c], in_=bank(pb), func=AF.Copy, scale=gff[:, c:c + 1]),
                            r=[("ps", pb), "const"], w=[("h2Tf", c)])
                        P.op("dve", lambda e, c=c, tt=tt: e.tensor_copy(
                            out=h2T[c][:, tt * 512:(tt + 1) * 512], in_=h2Tf[c]),
                            r=[("h2Tf", c)], w=[("h2T", c, tt)])
                    for j in range(4):
                        blk = tt * 4 + j
                        pb = 4 + j % 2
                        P.op("pe", lambda e, j=j, pb=pb: [
                            e.matmul(bank(pb)[:, 0:72], lhsT=h2Tf[c][:, j * 128:(j + 1) * 128], rhs=wrv[:, c, :],
                                     start=(c == 0), stop=(c == 7)) for c in range(8)],
                            r=[("h2Tf", c) for c in range(8)] + ["wr"], w=[("ps", pb)])
                        router_block(bank(pb)[:, 0:72], pb, blk)
                import os as _os
                for ex in range(int(_os.environ.get('K_NEXP', NEXP))):
                    sl = ex % 2
                    for (wsrc, wdst, wtag, si) in ((w_gate, Wg, "Wg", 0), (w_up, Wu, "Wu", 1)):
                        P.op("sp", lambda e, wsrc=wsrc, ex=ex, si=si: e.dma_start(
                            out=stgD[si].rearrange("p (c n) -> p c n", c=8),
                            in_=wsrc[ex].rearrange("(c p) n -> p c n", p=128)),
                            w=[("stgD", si)], semkey=("stgD", si))
                        P.op("pool", lambda e, wdst=wdst, si=si, sl=sl: e.tensor_copy(out=wdst[sl], in_=stgD[si]),
                             r=[("stgD", si)], w=[(wtag, sl)])
                    P.op("sp", lambda e, ex=ex: e.dma_start(
                        out=stgD[2].rearrange("p (c n) -> p c n", c=2),
                        in_=w_down[ex].rearrange("(c p) n -> p c n", p=128)),
                        w=[("stgD", 2)], semkey=("stgD", 2))
                    P.op("pool", lambda e, sl=sl: e.tensor_copy(out=Wd[sl], in_=stgD[2]),
                         r=[("stgD", 2)], w=[("Wd", sl)])
                    Wgv = Wg[sl].rearrange("p (c n) -> p c n", c=8)
                    Wuv = Wu[sl].rearrange("p (c n) -> p c n", c=8)
                    Wdv = Wd[sl].rearrange("p (c n) -> p c n", c=2)

                    def down(tt, ex=ex, sl=sl, Wdv=Wdv):
                        hs = hid[tt % 2]
                        for j in range(4):
                            blk = tt * 4 + j
                            for half in range(2):
                                pb = 4 + (j * 2 + half) % 4
                                P.op("pe", lambda e, j=j, half=half, pb=pb, hs=hs: [
                                    e.matmul(bank(pb), lhsT=hs[f][:, j * 128:(j + 1) * 128],
                                             rhs=Wdv[:, f, half * 512:(half + 1) * 512],
                                             start=(f == 0), stop=(f == 1)) for f in range(2)],
                                    r=[("hid", tt % 2, 0), ("hid", tt % 2, 1), ("Wd", sl)], w=[("ps", pb)])
                                P.op("dve", lambda e, blk=blk, half=half, pb=pb: e.scalar_tensor_tensor(
                                    out=acc[:, blk * 1024 + half * 512:blk * 1024 + (half + 1) * 512],
                                    in0=bank(pb), scalar=comb[:, blk * 64 + ex:blk * 64 + ex + 1],
                                    in1=acc[:, blk * 1024 + half * 512:blk * 1024 + (half + 1) * 512],
                                    op0=ALU.mult, op1=ALU.add),
                                    r=[("ps", pb), ("comb", blk), ("acc", blk, half)], w=[("acc", blk, half)])

                    for tt in range(NTS):
                        for f in range(2):
                            P.op("pe", lambda e, f=f, tt=tt, Wgv=Wgv: [
                                e.matmul(bank(f), lhsT=Wgv[:, c, f * 128:(f + 1) * 128],
                                         rhs=h2T[c][:, tt * 512:(tt + 1) * 512], start=(c == 0), stop=(c == 7))
                                for c in range(8)],
                                r=[("Wg", sl)] + [("h2T", c, tt) for c in range(8)], w=[("ps", f)])
                            P.op("pe", lambda e, f=f, tt=tt, Wuv=Wuv: [
                                e.matmul(bank(2 + f), lhsT=Wuv[:, c, f * 128:(f + 1) * 128],
                                         rhs=h2T[c][:, tt * 512:(tt + 1) * 512], start=(c == 0), stop=(c == 7))
                                for c in range(8)],
                                r=[("Wu", sl)] + [("h2T", c, tt) for c in range(8)], w=[("ps", 2 + f)])
                            P.op("act", lambda e, f=f: e.activation(out=sgt[f], in_=bank(f), func=AF.Silu),
                                 r=[("ps", f)], w=[("sgt", f)])
                            P.op("dve", lambda e, f=f, tt=tt: e.tensor_tensor(
                                out=hid[tt % 2][f], in0=sgt[f], in1=bank(2 + f), op=ALU.mult),
                                r=[("sgt", f), ("ps", 2 + f)], w=[("hid", tt % 2, f)])
                        if tt > 0:
                            down(tt - 1)
                    down(NTS - 1)
                for blk in range(NSB):
                    r0 = sti * ST + blk * 128
                    P.op("sp", lambda e, blk=blk, r0=r0: e.dma_start(
                        out=y[r0:r0 + 128, :], in_=acc[:, blk * 1024:(blk + 1) * 1024]),
                        r=[("acc", blk, 0), ("acc", blk, 1)], semkey=("yout", blk))
            P.barrier()
            A.reset()

        emit_layer(0, xb_in, False, xo_in, X1)
        for k in range(SO // CC_ROWS):
            P.op("pool", lambda e, k=k: e.collective_compute(
                "AllGather", ALU.bypass, replica_groups=[[0, 1, 2, 3], [4, 5, 6, 7]],
                ins=[X1[k * CC_ROWS:(k + 1) * CC_ROWS, :].opt()],
                outs=[XG[k * 4 * CC_ROWS:(k + 1) * 4 * CC_ROWS, :].opt()]),
                w=["XG"], semkey="cc", dmainc=1)
        P.barrier()
        emit_layer(1, XG, True, X1, y_out)
        P.emit(nc, stack)
    return nc


def _own_positions(S, r):
    nb = S // 512
    return np.concatenate([np.arange((4 * i + r) * 128, (4 * i + r + 1) * 128) for i in range(nb)])


def _rope_tables(pos):
    half = 32
    inv = (1.0 / (10000.0 ** (np.arange(0, 64, 2, dtype=np.float32) / np.float32(64)))).astype(np.float32)
    ang = pos.astype(np.float32)[:, None] * inv[None, :]
    cos = np.cos(ang).astype(np.float32)
    sin = np.sin(ang).astype(np.float32)
    idx = (np.arange(128) % 64) % half
    return np.ascontiguousarray(cos[:, idx].T), np.ascontiguousarray(sin[:, idx].T)


def _consts(S):
    bf = ml_dtypes.bfloat16
    identf = np.eye(128, dtype=np.float32)
    onesbd = np.zeros((128, 128), np.float32)
    onesbd[:64, :64] = 1
    onesbd[64:, 64:] = 1
    rot = np.zeros((128, 128), np.float32)
    for dp in range(128):
        if dp % 64 < 32:
            rot[dp + 32, dp] = -1.0
        else:
            rot[dp - 32, dp] = 1.0
    return {
        "identb": identf.astype(bf), "identf": identf, "onesbd": onesbd.astype(bf),
        "ones": np.ones((128, 128), np.float32).astype(bf), "rot": rot.astype(bf),
    }


def _mask(r):
    k = np.arange(128)[:, None, None]
    jb = np.arange(16)[None, :, None]
    q = np.arange(512)[None, None, :]
    keypos = jb * 128 + k
    qpos = (r + 4 * (q // 128)) * 128 + (q % 128)
    return (keypos <= qpos).astype(np.float32).reshape(128, 16 * 512).astype(ml_dtypes.bfloat16)


_PROG_CACHE = {}


def _get_prog(S):
    if S not in _PROG_CACHE:
        _PROG_CACHE[S] = build_program(S)
    return _PROG_CACHE[S]


def _col(v, n=128):
    return np.ascontiguousarray(np.asarray(v, np.float32).reshape(-1, n).T)


def _make_in_maps(x, inp):
    B, S, _ = x.shape
    consts = _consts(S)
    cosk, sink = _rope_tables(np.arange(S))
    f32 = np.float32
    shared = dict(consts)
    shared.update({
        "cosk": cosk, "sink": sink,
        "g_attn": np.concatenate([_col(inp["attn_norm"][0]), _col(inp["attn_norm"][1])], axis=1),
        "g_ffn": np.concatenate([_col(inp["ffn_norm"][0]), _col(inp["ffn_norm"][1])], axis=1),
        "w_in": inp["diff_w_in"][0],
        "gq": np.tile(inp["diff_q_norm"][0], 2)[:, None].astype(f32),
        "gk": np.tile(inp["diff_k_norm"][0], 2)[:, None].astype(f32),
        "lam4": np.stack([inp["diff_lambda_q1"][0], inp["diff_lambda_k1"][0],
                          inp["diff_lambda_q2"][0], inp["diff_lambda_k2"][0]]).astype(f32).reshape(1, 256),
        "subln": inp["diff_subln"][0][None, :].astype(f32),
        "w_a": inp["mla_w_a"][0], "w_qb": inp["mla_w_qb"][0], "w_kvb": inp["mla_w_kvb"][0],
        "g_qa": _col(inp["mla_q_a_norm"][0]), "g_kva": _col(inp["mla_kv_a_norm"][0]),
    })
    qn, kn = inp["mla_q_norm"][0].astype(f32), inp["mla_k_norm"][0].astype(f32)
    shared.update({
        "gqn": np.ascontiguousarray(qn[:128, None]), "gqr": np.ascontiguousarray(qn[128:, None]),
        "gkn": np.ascontiguousarray(kn[:128, None]), "gkr": np.ascontiguousarray(kn[128:, None]),
    })
    wouts = (inp["diff_w_out"][0], inp["mla_w_out"][0])
    for li in range(2):
        pf = "L%d_" % li
        shared.update({
            pf + "w_out": wouts[li],
            pf + "wr": np.ascontiguousarray(np.concatenate([inp["moe_w_group"][li], inp["moe_w_expert"][li]], axis=1)),
            pf + "br": np.concatenate([inp["moe_b_group"][li], inp["moe_b_expert"][li]])[None, :].astype(f32),
            pf + "w_gate": inp["moe_w_gate"][li], pf + "w_up": inp["moe_w_up"][li],
            pf + "w_down": inp["moe_w_down"][li],
        })
    in_maps, owns = [], []
    for c in range(NCORES):
        b, r = c // 4, c % 4
        own = _own_positions(S, r)
        owns.append((b, own))
        cosq, sinq = _rope_tables(own)
        m = dict(shared)
        m.update({"xb": np.ascontiguousarray(x[b]), "xo": np.ascontiguousarray(x[b][own]),
                  "cosq": cosq, "sinq": sinq, "maskd": _mask(r)})
        in_maps.append(m)
    return in_maps, owns


def run_fused(x, inp):
    B, S, _ = x.shape
    nc = _get_prog(S)
    in_maps, owns = _make_in_maps(x, inp)
    res = run_bass_kernel_spmd(nc, in_maps, core_ids=list(range(NCORES)))
    out = np.empty_like(x)
    for c in range(NCORES):
        b, own = owns[c]
        out[b][own] = res.results[c]["y"]
    return out


def kernel(**inputs):
    inp = {k: np.asarray(v) for k, v in inputs.items()}
    x = np.ascontiguousarray(inp["x"], dtype=np.float32)
    return run_fused(x, inp)
```

```python
import math
from contextlib import ExitStack
import numpy as np
import ml_dtypes
import concourse.bass as bass
import concourse.mybir as mybir
from concourse.bass_utils import run_bass_kernel_spmd

F32 = mybir.dt.float32
BF16 = mybir.dt.bfloat16
ALU = mybir.AluOpType
AF = mybir.ActivationFunctionType
AX = mybir.AxisListType

D = 1024
NH = 8
NEXP = 64
FF = 256
EPS = 1e-6
NCORES = 8
ST_MAX = 1024
CC_ROWS = 256


class _Op:
    __slots__ = ("eng", "fn", "deps", "dma", "semkey", "dmaval", "signal", "signo", "idx", "dmainc")


class Prog:
    COMPUTE = ("pe", "act", "dve", "pool")
    STREAMS = ("pe", "act", "dve", "pool", "sp")

    def __init__(self):
        self.ops = []
        self.lastw = {}
        self.readers = {}
        self.dma_cnt = {}
        self.last_on = {}
        self.dma_since_barrier = []

    def op(self, eng, fn, r=(), w=(), semkey=None, ndma=1, dmainc=16):
        o = _Op()
        o.dmainc = dmainc
        o.eng = eng
        o.fn = fn
        o.dma = semkey is not None
        o.semkey = semkey
        o.signal = False
        o.signo = 0
        o.dmaval = 0
        o.idx = len(self.ops)
        deps = set()
        for x in r:
            lw = self.lastw.get(x)
            if lw is not None:
                deps.add(lw)
        for x in w:
            lw = self.lastw.get(x)
            if lw is not None:
                deps.add(lw)
            rd = self.readers.get(x)
            if rd:
                for v in rd[0].values():
                    deps.add(v)
                for v in rd[1]:
                    deps.add(v)
        o.deps = deps
        for x in w:
            self.lastw[x] = o
            self.readers[x] = ({}, [])
        for x in r:
            rd = self.readers.get(x)
            if rd is None:
                rd = ({}, [])
                self.readers[x] = rd
            if o.dma:
                rd[1].append(o)
            else:
                rd[0][eng] = o
        if o.dma:
            c = self.dma_cnt.get(semkey, 0) + ndma
            self.dma_cnt[semkey] = c
            o.dmaval = dmainc * c
            self.dma_since_barrier.append(o)
        else:
            self.last_on[eng] = o
        self.ops.append(o)
        return o

    def barrier(self):
        deps = set(self.last_on.values()) | set(self.dma_since_barrier)
        self.dma_since_barrier = []
        for e in self.STREAMS:
            o = _Op()
            o.dmainc = 16
            o.eng = e
            o.fn = None
            o.dma = False
            o.semkey = None
            o.signal = False
            o.signo = 0
            o.dmaval = 0
            o.idx = len(self.ops)
            o.deps = set(deps)
            self.ops.append(o)
        self.lastw = {}
        self.readers = {}

    def emit(self, nc, stack):
        for o in self.ops:
            for d in o.deps:
                if d.dma:
                    continue
                if d.eng == "pe" and o.eng == "pe" and not o.dma:
                    continue
                d.signal = True
        cnt = {e: 0 for e in self.COMPUTE}
        for o in self.ops:
            if o.signal:
                cnt[o.eng] += 1
                o.signo = cnt[o.eng]
        sems = {e: stack.enter_context(nc.semaphore("s_" + e)) for e in self.COMPUTE}
        dsem = {}
        for k in self.dma_cnt:
            dsem[k] = stack.enter_context(nc.semaphore("d_%d" % len(dsem)))
        per = {e: [] for e in self.STREAMS}
        for o in self.ops:
            per[o.eng].append(o)
        block = stack.enter_context(nc.Block())

        def run(stream, eng):
            waited = {}
            for o in per[stream]:
                need = {}
                for d in o.deps:
                    if d.dma:
                        key = ("d", d.semkey)
                        val = d.dmaval
                    else:
                        if d.eng == "pe" and o.eng == "pe" and not o.dma:
                            continue
                        key = ("c", d.eng)
                        val = d.signo
                    if val > need.get(key, 0):
                        need[key] = val
                for key, val in need.items():
                    if waited.get(key, 0) >= val:
                        continue
                    waited[key] = val
                    s = dsem[key[1]] if key[0] == "d" else sems[key[1]]
                    eng.wait_ge(s, val)
                if o.fn is None:
                    continue
                ins = o.fn(eng)
                if o.dma:
                    if isinstance(ins, (list, tuple)):
                        for i_ in ins:
                            i_.then_inc(dsem[o.semkey], o.dmainc)
                    else:
                        ins.then_inc(dsem[o.semkey], o.dmainc)
                elif o.signal:
                    if isinstance(ins, (list, tuple)):
                        ins = ins[-1]
                    ins.then_inc(sems[o.eng], 1)

        @block.sync
        def _(e):
            run("sp", e)

        @block.tensor
        def _(e):
            run("pe", e)

        @block.scalar
        def _(e):
            run("act", e)

        @block.vector
        def _(e):
            run("dve", e)

        @block.gpsimd
        def _(e):
            run("pool", e)


class Arena:
    def __init__(self, ap, ncols):
        self.ap = ap
        self.ncols = ncols
        self.base = 0
        self.cur = 0

    def mark(self):
        self.base = self.cur

    def reset(self):
        self.cur = self.base

    def get(self, ncols, dt=BF16):
        if dt == F32:
            self.cur = (self.cur + 1) // 2 * 2
            n = 2 * ncols
        else:
            n = ncols
        n = (n + 1) // 2 * 2
        a = self.ap[:, self.cur:self.cur + n]
        self.cur += n
        assert self.cur <= self.ncols, ("SBUF arena overflow", self.cur, self.ncols)
        if dt == F32:
            return a.bitcast(F32)
        return a


def build_program(S):
    NKB = S // 128
    NT = S // 512
    SO = S // 4
    NTO = SO // 512
    NG = NTO
    ST = min(ST_MAX, SO)
    nc = bass.Bass("TRN2", target_bir_lowering=False)
    P = Prog()

    def din(name, shape, dt=F32):
        return nc.dram_tensor(name, list(shape), dt, kind="ExternalInput").ap()

    def dscr(name, shape, dt=BF16):
        return nc.dram_tensor(name, list(shape), dt, kind="Internal").ap()

    xb_in = din("xb", [S, D])
    xo_in = din("xo", [SO, D])
    y_out = nc.dram_tensor("y", [SO, D], F32, kind="ExternalOutput").ap()
    X1 = dscr("X1", [SO, D], F32)
    XG = dscr("XG", [4 * SO, D], F32)
    cosk = din("cosk", [128, S])
    sink = din("sink", [128, S])
    cosq = din("cosq", [128, SO])
    sinq = din("sinq", [128, SO])
    maskd = din("maskd", [128, 16 * 512], BF16)
    identb_d = din("identb", [128, 128], BF16)
    identf_d = din("identf", [128, 128])
    onesbd_d = din("onesbd", [128, 128], BF16)
    ones_d = din("ones", [128, 128], BF16)
    rot_d = din("rot", [128, 128], BF16)
    g_attn_all = din("g_attn", [128, 16])
    g_ffn_all = din("g_ffn", [128, 16])
    LW = {}
    for kd in (0, 1):
        pf = "L%d_" % kd
        LW[kd] = dict(
            w_out=din(pf + "w_out", [D, D]), wr=din(pf + "wr", [D, 72]), br=din(pf + "br", [1, 72]),
            w_gate=din(pf + "w_gate", [NEXP, D, FF]), w_up=din(pf + "w_up", [NEXP, D, FF]),
            w_down=din(pf + "w_down", [NEXP, FF, D]))
    w_in = din("w_in", [D, 3 * D])
    gq = din("gq", [128, 1])
    gk = din("gk", [128, 1])
    lam4 = din("lam4", [1, 256])
    subln = din("subln", [1, 128])
    w_a = din("w_a", [D, 704])
    g_qa = din("g_qa", [128, 3])
    g_kva = din("g_kva", [128, 2])
    w_qb = din("w_qb", [384, 1536])
    w_kvb = din("w_kvb", [256, 2048])
    gqn = din("gqn", [128, 1])
    gqr = din("gqr", [64, 1])
    gkn = din("gkn", [128, 1])
    gkr = din("gkr", [64, 1])
    KT = dscr("KT", [NH, 128, S])
    KR = dscr("KR", [NH, 64, S])
    QT = dscr("QT", [NH, 128, SO])
    QR = dscr("QR", [NH, 64, SO])
    VS = dscr("VS", [NH, 128, NKB, 128])
    AT = dscr("AT", [NH, 128, SO])

    stack = ExitStack()
    with stack:
        ARENA_COLS = 94 * 1024
        arena_t = stack.enter_context(nc.sbuf_tensor("arena", [128, ARENA_COLS], BF16))
        A = Arena(arena_t[:], ARENA_COLS)
        ps_t = stack.enter_context(nc.psum_tensor("ps", [128, 4096], F32))
        ps = ps_t[:]

        def bank(i, n=512, off=0):
            return ps[:, i * 512 + off:i * 512 + off + n]

        def bank_bf(i):
            return ps[:, i * 512:(i + 1) * 512].bitcast(BF16)

        identb = A.get(128)
        identf = A.get(128, F32)
        onesbd = A.get(128)
        ones = A.get(128)
        rot = A.get(128)
        gat_all = A.get(16, F32)
        gff_all = A.get(16, F32)
        junk = A.get(1024)
        small = A.get(64, F32)
        P.op("sp", lambda e: [
            e.dma_start(out=identb, in_=identb_d[:, :]),
            e.dma_start(out=identf, in_=identf_d[:, :]),
            e.dma_start(out=onesbd, in_=onesbd_d[:, :]),
            e.dma_start(out=ones, in_=ones_d[:, :]),
            e.dma_start(out=rot, in_=rot_d[:, :]),
            e.dma_start(out=gat_all, in_=g_attn_all[:, :]),
            e.dma_start(out=gff_all, in_=g_ffn_all[:, :]),
        ], w=["const"], semkey="const", ndma=7)
        A.mark()

        def emit_layer(kind, xb, xb_gathered, xo, y):
            gat = gat_all[:, kind * 8:(kind + 1) * 8]
            gff = gff_all[:, kind * 8:(kind + 1) * 8]
            w_out, wr, br = LW[kind]["w_out"], LW[kind]["wr"], LW[kind]["br"]
            w_gate, w_up, w_down = LW[kind]["w_gate"], LW[kind]["w_up"], LW[kind]["w_down"]
            def load_x_tile(src, t, xt, tag, extra_w=(), gathered=False):
                if gathered:
                    k_, o_ = (t * 128) // CC_ROWS, (t * 128) % CC_ROWS
                    in_ap = src.rearrange("(k j n) d -> k j n d", j=4, n=CC_ROWS)[k_, :, o_:o_ + 128, :].rearrange(
                        "j p d -> p j d")
                else:
                    in_ap = src[t * 512:(t + 1) * 512, :].rearrange("(j p) d -> p j d", p=128)
                P.op("sp", lambda e: e.dma_start(
                    out=xt.rearrange("p (j d) -> p j d", j=4), in_=in_ap),
                    w=[tag] + list(extra_w), semkey=tag)

            def norm_tile(xt, xtag, hb, hbtag, st4, sttag):
                for j in range(4):
                    P.op("act", lambda e, j=j: e.activation(
                        out=junk, in_=xt[:, j * 1024:(j + 1) * 1024], func=AF.Square,
                        accum_out=st4[:, j:j + 1]), r=[xtag, "const"], w=["junk", (sttag, j)])
                P.op("act", lambda e: e.activation(out=st4[:, 4:8], in_=st4[:, 0:4], func=AF.Ln,
                                                   scale=1.0 / D, bias=EPS),
                     r=[(sttag, j) for j in range(4)], w=[(sttag, "ln")])
                P.op("act", lambda e: e.activation(out=st4[:, 8:12], in_=st4[:, 4:8], func=AF.Exp,
                                                   scale=-0.5),
                     r=[(sttag, "ln")], w=[(sttag, "rs")])
                for j in range(4):
                    P.op("dve", lambda e, j=j: e.tensor_scalar(
                        out=hb[:, j * 1024:(j + 1) * 1024], in0=xt[:, j * 1024:(j + 1) * 1024],
                        scalar1=st4[:, 8 + j:9 + j], scalar2=None, op0=ALU.mult),
                        r=[xtag, (sttag, "rs")], w=[(hbtag, j)])

            def transpose_tile_bf(hb, hbtag, hT, hTtag, gains, psb):
                for c in range(8):
                    pb = psb[c % 2]
                    pt = bank_bf(pb)[:, 0:512]
                    P.op("pe", lambda e, c=c, pt=pt: [
                        e.transpose(out=pt[:, j * 128:(j + 1) * 128],
                                    in_=hb[:, j * 1024 + c * 128:j * 1024 + (c + 1) * 128],
                                    identity=identb) for j in range(4)],
                        r=[(hbtag, j) for j in range(4)] + ["const"], w=[("ps", pb)])
                    eng = "act" if c % 2 == 0 else "dve"
                    if eng == "act":
                        P.op("act", lambda e, c=c, pt=pt: e.activation(
                            out=hT[c], in_=pt, func=AF.Copy, scale=gains[:, c:c + 1]),
                            r=[("ps", pb), "const"], w=[(hTtag, c)])
                    else:
                        P.op("dve", lambda e, c=c, pt=pt: e.tensor_scalar(
                            out=hT[c], in0=pt, scalar1=gains[:, c:c + 1], scalar2=None, op0=ALU.mult),
                            r=[("ps", pb), "const"], w=[(hTtag, c)])

            def load_cast_weight(dst, dsttag, src_rows, nrows_chunks, ncols, stg, stgtag, col0=0,
                                 engs=("pool", "dve")):
                for c in range(nrows_chunks):
                    s = stg[c % len(stg)]
                    stag = (stgtag, c % len(stg))
                    P.op("sp", lambda e, c=c, s=s: e.dma_start(
                        out=s[:, 0:ncols], in_=src_rows[c * 128:(c + 1) * 128, col0:col0 + ncols]),
                        w=[stag], semkey=stag)
                    eng = engs[c % len(engs)]
                    P.op(eng, lambda e, c=c, s=s: e.tensor_copy(
                        out=dst[:, c * ncols:(c + 1) * ncols], in_=s[:, 0:ncols]),
                        r=[stag], w=[(dsttag, c)])

            def feature_head_post(psrc, pb_src, nparts, g_col, cos_t, sin_t, tabtag, out_bf, outtag,
                                  rstd, rstdtag, tmp, tmptag, psrot):
                kgb, t1, t2 = tmp
                P.op("act", lambda e: e.activation(out=kgb[0:nparts, :], in_=psrc, func=AF.Copy,
                                                   scale=g_col),
                     r=[("ps", pb_src), "const", "gains"], w=[(tmptag, "kgb")])
                pr = bank(psrot)[0:nparts, :]
                P.op("pe", lambda e: e.matmul(pr, lhsT=rot[0:nparts, 0:nparts], rhs=kgb[0:nparts, :],
                                              start=True, stop=True),
                     r=[(tmptag, "kgb"), "const"], w=[("ps", psrot)])
                P.op("dve", lambda e: e.tensor_tensor(out=t1[0:nparts, :], in0=kgb[0:nparts, :],
                                                      in1=cos_t[0:nparts, :], op=ALU.mult),
                     r=[(tmptag, "kgb"), tabtag], w=[(tmptag, "t1")])
                P.op("dve", lambda e: e.tensor_tensor(out=t2[0:nparts, :], in0=pr,
                                                      in1=sin_t[0:nparts, :], op=ALU.mult),
                     r=[("ps", psrot), tabtag], w=[(tmptag, "t2")])
                if rstd is None:
                    P.op("pool", lambda e: e.tensor_tensor(out=out_bf, in0=t1[0:nparts, :],
                                                           in1=t2[0:nparts, :], op=ALU.add),
                         r=[(tmptag, "t2"), (tmptag, "t1")], w=[outtag])
                    return
                P.op("pool", lambda e: e.tensor_tensor(out=t1[0:nparts, :], in0=t1[0:nparts, :],
                                                       in1=t2[0:nparts, :], op=ALU.add),
                     r=[(tmptag, "t2"), (tmptag, "t1")], w=[(tmptag, "t1")])
                P.op("pool", lambda e: e.tensor_tensor(out=out_bf, in0=t1[0:nparts, :],
                                                       in1=rstd[0:nparts, :], op=ALU.mult),
                     r=[(tmptag, "t1"), rstdtag], w=[outtag])

            def rstd_from_ps(psms, pb, rstd, rstdtag, lnt, n):
                P.op("act", lambda e: e.activation(out=lnt, in_=psms, func=AF.Ln, scale=1.0 / n, bias=EPS),
                     r=[("ps", pb)], w=[(rstdtag, "ln")])
                P.op("act", lambda e: e.activation(out=rstd, in_=lnt, func=AF.Exp, scale=-0.5),
                     r=[(rstdtag, "ln")], w=[rstdtag])

            if kind == 0:
                Wb = A.get(8 * 3072)
                stg = [A.get(3072, F32), A.get(3072, F32)]
                gqk = A.get(2, F32)
                P.op("sp", lambda e: [e.dma_start(out=gqk[:, 0:1], in_=gq[:, :]),
                                      e.dma_start(out=gqk[:, 1:2], in_=gk[:, :])],
                     w=["gains"], semkey="gains", ndma=2)
                load_cast_weight(Wb, "Wb", w_in, 8, 3072, stg, "stg")
                Wv = Wb.rearrange("p (c n) -> p c n", c=8)
                xts = [A.get(4096, F32), A.get(4096, F32)]
                hb = A.get(4096)
                hT = [A.get(512) for _ in range(8)]
                st4 = A.get(16, F32)
                tabs = [(A.get(512, F32), A.get(512, F32)) for _ in range(2)]
                sq2 = [A.get(512), A.get(512)]
                tmp2 = [(A.get(512), A.get(512, F32), A.get(512, F32)) for _ in range(2)]
                lnt2 = [A.get(512, F32), A.get(512, F32)]
                rstd2 = [A.get(512, F32), A.get(512, F32)]
                kf = [A.get(512), A.get(512)]
                vb = A.get(4096)

                def proj_phase(src, ntiles, cos_d, sin_d, do_kv, toff):
                    for t in range(ntiles):
                        xt = xts[(t + toff) % 2]
                        xtag = ("x", (t + toff) % 2)
                        load_x_tile(src, t, xt, xtag, gathered=(do_kv and xb_gathered))
                        tb = tabs[(t + toff) % 2]
                        ttag = ("tab", (t + toff) % 2)
                        P.op("sp", lambda e, t=t, tb=tb: [
                            e.dma_start(out=tb[0], in_=cos_d[:, t * 512:(t + 1) * 512]),
                            e.dma_start(out=tb[1], in_=sin_d[:, t * 512:(t + 1) * 512])],
                            w=[ttag], semkey=ttag, ndma=2)
                        norm_tile(xt, xtag, hb, "hb", st4, "st4")
                        transpose_tile_bf(hb, "hb", hT, "hT", gat, (0, 1))
                        heads = range(NH)
                        vpend = []
                        if do_kv:
                            for j in range(4):
                                for half in range(2):
                                    def vgroup(j=j, half=half, t=t):
                                        pv = 6 + (half % 2)
                                        P.op("pe", lambda e: [
                                            e.matmul(bank(pv), lhsT=hT[c][:, j * 128:(j + 1) * 128],
                                                     rhs=Wv[:, c, 2 * D + half * 512:2 * D + (half + 1) * 512],
                                                     start=(c == 0), stop=(c == 7)) for c in range(8)],
                                            r=[("Wb", c) for c in range(8)] + [("hT", c) for c in range(8)],
                                            w=[("ps", pv)])
                                        P.op("dve", lambda e: e.tensor_copy(
                                            out=vb[:, j * 1024 + half * 512:j * 1024 + (half + 1) * 512],
                                            in_=bank(pv)), r=[("ps", pv)], w=[("vb", j, half)])
                                        if half == 1:
                                            P.op("pool", lambda e: e.dma_start(
                                                out=VS[:, :, t * 4 + j, :].rearrange("h p d -> p h d"),
                                                in_=vb[:, j * 1024:(j + 1) * 1024].rearrange("p (h d) -> p h d", h=NH)),
                                                r=[("vb", j, 0), ("vb", j, 1)], semkey=("vst", j))
                                    vpend.append(vgroup)
                        for h in heads:
                            col0 = (D + h * 128) if do_kv else (h * 128)
                            pk = 2 + (h % 2)
                            pkt = bank(pk)
                            P.op("pe", lambda e, col0=col0, pkt=pkt: [
                                e.matmul(pkt, lhsT=Wv[:, c, col0:col0 + 128], rhs=hT[c],
                                         start=(c == 0), stop=(c == 7)) for c in range(8)],
                                r=[("Wb", c) for c in range(8)] + [("hT", c) for c in range(8)],
                                w=[("ps", pk)])
                            if vpend:
                                vpend.pop(0)()
                            hp = h % 2
                            sq, tmp, lnt, rstd = sq2[hp], tmp2[hp], lnt2[hp], rstd2[hp]
                            P.op("act", lambda e, pkt=pkt, sq=sq: e.activation(out=sq, in_=pkt, func=AF.Square),
                                 r=[("ps", pk)], w=[("sq", hp)])
                            P.op("pe", lambda e, sq=sq: e.matmul(bank(4), lhsT=onesbd, rhs=sq, start=True, stop=True),
                                 r=[("sq", hp), "const"], w=[("ps", 4)])
                            rstd_from_ps(bank(4), 4, rstd, ("rstd", hp), lnt, 64)
                            kfb = kf[h % 2]
                            kftag = ("kf", h % 2)
                            feature_head_post(pkt, pk, 128, gqk[:, (1 if do_kv else 0):(2 if do_kv else 1)],
                                              tb[0], tb[1], ttag, kfb, kftag, rstd, ("rstd", hp), tmp, ("tmp", hp), 5)
                            dst = KT if do_kv else QT
                            P.op("pool", lambda e, h=h, t=t, kfb=kfb, dst=dst: e.dma_start(
                                out=dst[h, :, t * 512:(t + 1) * 512], in_=kfb),
                                r=[kftag], semkey=("kst", h % 2))
                        while vpend:
                            vpend.pop(0)()

                proj_phase(xb, NT, cosk, sink, True, 0)
                proj_phase(xo, NTO, cosq, sinq, False, NT)
            else:
                Wa = A.get(8 * 704)
                Wav = Wa.rearrange("p (c n) -> p c n", c=8)
                Wk = A.get(2 * 1024)
                Wkv_ = Wk.rearrange("p (f n) -> p f n", f=2)
                Wvv = A.get(2 * 1024)
                Wvv_ = Wvv.rearrange("p (f n) -> p f n", f=2)
                Wqb = A.get(3 * 1536)
                Wqv = Wqb.rearrange("p (f n) -> p f n", f=3)
                stg = [A.get(3072, F32), A.get(3072, F32)]
                gt = A.get(16, F32)
                P.op("sp", lambda e: [e.dma_start(out=gt[:, 0:3], in_=g_qa[:, :]),
                                      e.dma_start(out=gt[:, 3:5], in_=g_kva[:, :]),
                                      e.dma_start(out=gt[:, 5:6], in_=gqn[:, :]),
                                      e.dma_start(out=gt[0:64, 6:7], in_=gqr[:, :]),
                                      e.dma_start(out=gt[:, 7:8], in_=gkn[:, :]),
                                      e.dma_start(out=gt[0:64, 8:9], in_=gkr[:, :])],
                     w=["gains"], semkey="gains", ndma=6)
                load_cast_weight(Wa, "Wa", w_a, 8, 704, stg, "stg")
                load_cast_weight(Wqb, "Wqb", w_qb, 3, 1536, stg, "stg")
                for f in range(2):
                    s_ = stg[f % 2]
                    stag = ("stg", f % 2)
                    P.op("sp", lambda e, f=f, s_=s_: e.dma_start(out=s_[:, 0:2048], in_=w_kvb[f * 128:(f + 1) * 128, :]),
                         w=[stag], semkey=stag)
                    sv_ = s_[:, 0:2048].rearrange("p (h t d) -> p h t d", h=8, t=2)
                    P.op("pool", lambda e, f=f, sv_=sv_: e.tensor_copy(
                        out=Wk[:, f * 1024:(f + 1) * 1024].rearrange("p (h d) -> p h d", h=8), in_=sv_[:, :, 0, :]),
                        r=[stag], w=[("Wk", f)])
                    P.op("dve", lambda e, f=f, sv_=sv_: e.tensor_copy(
                        out=Wvv[:, f * 1024:(f + 1) * 1024].rearrange("p (h d) -> p h d", h=8), in_=sv_[:, :, 1, :]),
                        r=[stag], w=[("Wv", f)])
                xts = [A.get(4096, F32), A.get(4096, F32)]
                hb = A.get(4096)
                hT = [A.get(512) for _ in range(8)]
                st4 = A.get(16, F32)
                tabs = [(A.get(512, F32), A.get(512, F32)) for _ in range(2)]
                sqc = [A.get(512) for _ in range(3)]
                cn = [A.get(512) for _ in range(3)]
                sq2 = [A.get(512), A.get(512)]
                sqr2 = [A.get(512), A.get(512)]
                sqr = A.get(512)
                tmp2 = [(A.get(512), A.get(512, F32), A.get(512, F32)) for _ in range(2)]
                tmp = (A.get(512), A.get(512, F32), A.get(512, F32))
                lnt2 = [A.get(512, F32), A.get(512, F32)]
                lnt = A.get(512, F32)
                rstd2 = [A.get(512, F32), A.get(512, F32)]
                rstdc = A.get(512, F32)
                rp = A.get(512, F32)
                kf = [A.get(512), A.get(512)]
                krf = [A.get(512), A.get(512)]
                vb = A.get(4096)

                def mla_phase(src, ntiles, cos_d, sin_d, do_kv, toff):
                    for t in range(ntiles):
                        xt = xts[(t + toff) % 2]
                        xtag = ("x", (t + toff) % 2)
                        load_x_tile(src, t, xt, xtag, gathered=(do_kv and xb_gathered))
                        tb = tabs[(t + toff) % 2]
                        ttag = ("tab", (t + toff) % 2)
                        P.op("sp", lambda e, t=t, tb=tb: [
                            e.dma_start(out=tb[0], in_=cos_d[:, t * 512:(t + 1) * 512]),
                            e.dma_start(out=tb[1], in_=sin_d[:, t * 512:(t + 1) * 512])],
                            w=[ttag], semkey=ttag, ndma=2)
                        norm_tile(xt, xtag, hb, "hb", st4, "st4")
                        transpose_tile_bf(hb, "hb", hT, "hT", gat, (0, 1))
                        hTr = [("hT", c) for c in range(8)]
                        War = [("Wa", c) for c in range(8)]
                        nf = 2 if do_kv else 3
                        col0 = 384 if do_kv else 0
                        gcol0 = 3 if do_kv else 0
                        cbanks = (2, 3, 5)
                        for f in range(nf):
                            pb = cbanks[f]
                            P.op("pe", lambda e, f=f, pb=pb: [
                                e.matmul(bank(pb), lhsT=Wav[:, c, col0 + f * 128:col0 + (f + 1) * 128], rhs=hT[c],
                                         start=(c == 0), stop=(c == 7)) for c in range(8)],
                                r=War + hTr, w=[("ps", pb)])
                            P.op("act", lambda e, f=f, pb=pb: e.activation(out=sqc[f], in_=bank(pb), func=AF.Square),
                                 r=[("ps", pb)], w=[("sqc", f)])
                        P.op("pe", lambda e: [e.matmul(bank(4), lhsT=ones, rhs=sqc[f], start=(f == 0), stop=(f == nf - 1))
                                              for f in range(nf)],
                             r=[("sqc", f) for f in range(nf)] + ["const"], w=[("ps", 4)])
                        rstd_from_ps(bank(4), 4, rstdc, "rstdc", lnt, 128 * nf)
                        for f in range(nf):
                            pb = cbanks[f]
                            P.op("dve", lambda e, f=f, pb=pb: e.scalar_tensor_tensor(
                                out=cn[f], in0=bank(pb), scalar=gt[:, gcol0 + f:gcol0 + f + 1], in1=rstdc,
                                op0=ALU.mult, op1=ALU.mult),
                                r=[("ps", pb), "gains", "rstdc"], w=[("cn", f)])
                        cnr = [("cn", f) for f in range(nf)]
                        if do_kv:
                            P.op("pe", lambda e: [
                                e.matmul(bank(5)[0:64, :], lhsT=Wav[:, c, 640:704], rhs=hT[c],
                                         start=(c == 0), stop=(c == 7)) for c in range(8)],
                                r=War + hTr, w=[("ps", 5)])
                            P.op("act", lambda e: e.activation(out=sqr[0:64, :], in_=bank(5)[0:64, :], func=AF.Square),
                                 r=[("ps", 5)], w=["sqr"])
                            feature_head_post(bank(5)[0:64, :], 5, 64, gt[0:64, 8:9], tb[0], tb[1], ttag,
                                              rp[0:64, :], "rp", None, None, tmp, "tmp", 6)
                        for h in range(NH):
                            pk = 2 + (h % 2)
                            hp = h % 2
                            sq, lnt_h, rstd = sq2[hp], lnt2[hp], rstd2[hp]
                            sqr_h = sqr if do_kv else sqr2[hp]
                            sqrtag = "sqr" if do_kv else ("sqr", hp)
                            tmp_h = tmp2[hp]
                            if do_kv:
                                P.op("pe", lambda e, h=h, pk=pk: [
                                    e.matmul(bank(pk), lhsT=Wkv_[:, f, h * 128:(h + 1) * 128], rhs=cn[f],
                                             start=(f == 0), stop=(f == 1)) for f in range(2)],
                                    r=cnr + [("Wk", 0), ("Wk", 1)], w=[("ps", pk)])
                            else:
                                P.op("pe", lambda e, h=h, pk=pk: [
                                    e.matmul(bank(pk), lhsT=Wqv[:, f, h * 192:h * 192 + 128], rhs=cn[f],
                                             start=(f == 0), stop=(f == 2)) for f in range(3)],
                                    r=cnr + [("Wqb", f) for f in range(3)], w=[("ps", pk)])
                                P.op("pe", lambda e, h=h: [
                                    e.matmul(bank(5)[0:64, :], lhsT=Wqv[:, f, h * 192 + 128:h * 192 + 192], rhs=cn[f],
                                             start=(f == 0), stop=(f == 2)) for f in range(3)],
                                    r=cnr + [("Wqb", f) for f in range(3)], w=[("ps", 5)])
                                P.op("act", lambda e, sqr_h=sqr_h: e.activation(
                                    out=sqr_h[0:64, :], in_=bank(5)[0:64, :], func=AF.Square),
                                     r=[("ps", 5)], w=[sqrtag])
                            P.op("act", lambda e, pk=pk, sq=sq: e.activation(out=sq, in_=bank(pk), func=AF.Square),
                                 r=[("ps", pk)], w=[("sq", hp)])
                            P.op("pe", lambda e, sq=sq, sqr_h=sqr_h: [
                                e.matmul(bank(4), lhsT=ones, rhs=sq, start=True, stop=False),
                                e.matmul(bank(4), lhsT=ones[0:64, :], rhs=sqr_h[0:64, :], start=False, stop=True)],
                                r=[("sq", hp), sqrtag, "const"], w=[("ps", 4)])
                            rstd_from_ps(bank(4), 4, rstd, ("rstd", hp), lnt_h, 192)
                            kfb = kf[h % 2]
                            kftag = ("kf", h % 2)
                            gc = 7 if do_kv else 5
                            P.op("dve", lambda e, pk=pk, kfb=kfb, gc=gc, rstd=rstd: e.scalar_tensor_tensor(
                                out=kfb, in0=bank(pk), scalar=gt[:, gc:gc + 1], in1=rstd, op0=ALU.mult, op1=ALU.mult),
                                r=[("ps", pk), "gains", ("rstd", hp)], w=[kftag])
                            dstn = KT if do_kv else QT
                            P.op("pool", lambda e, h=h, t=t, kfb=kfb, dstn=dstn: e.dma_start(
                                out=dstn[h, :, t * 512:(t + 1) * 512], in_=kfb),
                                r=[kftag], semkey=("kst", h % 2))
                            krb = krf[h % 2]
                            krtag = ("krf", h % 2)
                            if do_kv:
                                P.op("pool", lambda e, krb=krb, rstd=rstd: e.tensor_tensor(
                                    out=krb[0:64, :], in0=rp[0:64, :], in1=rstd[0:64, :], op=ALU.mult),
                                    r=["rp", ("rstd", hp)], w=[krtag])
                            else:
                                feature_head_post(bank(5)[0:64, :], 5, 64, gt[0:64, 6:7], tb[0], tb[1], ttag,
                                                  krb[0:64, :], krtag, rstd, ("rstd", hp), tmp_h, ("tmp", hp), 6)
                            dstr = KR if do_kv else QR
                            P.op("pool", lambda e, h=h, t=t, krb=krb, dstr=dstr: e.dma_start(
                                out=dstr[h, :, t * 512:(t + 1) * 512], in_=krb[0:64, :]),
                                r=[krtag], semkey=("krst", h % 2))
                        if do_kv:
                            for j in range(4):
                                for half in range(2):
                                    pv = 7
                                    P.op("pe", lambda e, j=j, half=half, pv=pv: [
                                        e.matmul(bank(pv), lhsT=cn[f][:, j * 128:(j + 1) * 128],
                                                 rhs=Wvv_[:, f, half * 512:(half + 1) * 512],
                                                 start=(f == 0), stop=(f == 1)) for f in range(2)],
                                        r=cnr + [("Wv", 0), ("Wv", 1)], w=[("ps", pv)])
                                    P.op("act", lambda e, j=j, half=half, pv=pv: e.activation(
                                        out=vb[:, j * 1024 + half * 512:j * 1024 + (half + 1) * 512],
                                        in_=bank(pv), func=AF.Copy), r=[("ps", pv)], w=[("vb", j, half)])
                                P.op("pool", lambda e, j=j, t=t: e.dma_start(
                                    out=VS[:, :, t * 4 + j, :].rearrange("h p d -> p h d"),
                                    in_=vb[:, j * 1024:(j + 1) * 1024].rearrange("p (h d) -> p h d", h=NH)),
                                    r=[("vb", j, 0), ("vb", j, 1)], semkey=("vst", j))

                mla_phase(xb, NT, cosk, sink, True, 0)
                mla_phase(xo, NTO, cosq, sinq, False, NT)

            P.barrier()
            A.reset()

            NCH = NKB // 16
            Kt = A.get(S)
            Vt = A.get(NKB * 130)
            Vv = Vt.rearrange("p (k d) -> p k d", d=130)
            Qt = A.get(SO)
            if kind == 1:
                Kr = A.get(S)
                Qr = A.get(SO)
            msk = A.get(16 * 512)
            Pt = [A.get(1024), A.get(1024)]
            o1 = A.get(128, F32)
            oo = A.get(128, F32)
            attT = A.get(512)
            sg = A.get(128, F32)
            lamt = A.get(4 * 64, F32)
            cst = A.get(16, F32)
            P.op("sp", lambda e: e.dma_start(out=msk, in_=maskd[:, :]), w=["msk"], semkey="msk")
            P.op("pool", lambda e: e.memset(Vv[:, :, 128:130], 1.0), w=[("V", c) for c in range(NCH)])
            if kind == 0:
                lam_init = 0.8 - 0.6 * math.exp(-0.3 * 0)
                P.op("sp", lambda e: [e.dma_start(out=sg, in_=subln.partition_broadcast(128)),
                                      e.dma_start(out=lamt, in_=lam4.partition_broadcast(128))],
                     w=["sg", "lamt"], semkey="cC", ndma=2)
                P.op("dve", lambda e: e.tensor_scalar(out=sg, in0=sg, scalar1=1.0 - lam_init, scalar2=None,
                                                      op0=ALU.mult), r=["sg"], w=["sg"])
                P.op("dve", lambda e: e.tensor_tensor(out=lamt[:, 0:64], in0=lamt[:, 0:64], in1=lamt[:, 64:128],
                                                      op=ALU.mult), r=["lamt"], w=["lam_a"])
                P.op("dve", lambda e: e.tensor_tensor(out=lamt[:, 128:192], in0=lamt[:, 128:192],
                                                      in1=lamt[:, 192:256], op=ALU.mult), r=["lamt"], w=["lam_b"])
                P.op("dve", lambda e: e.reduce_sum(out=cst[:, 0:1], in_=lamt[:, 0:64], axis=AX.X),
                     r=["lam_a"], w=["lam_s1"])
                P.op("dve", lambda e: e.reduce_sum(out=cst[:, 1:2], in_=lamt[:, 128:192], axis=AX.X),
                     r=["lam_b"], w=["lam_s2"])
                P.op("act", lambda e: e.activation(out=cst[:, 2:4], in_=cst[:, 0:2], func=AF.Exp),
                     r=["lam_s1", "lam_s2"], w=["lam_e"])
                P.op("dve", lambda e: e.tensor_tensor(out=cst[:, 4:5], in0=cst[:, 3:4], in1=cst[:, 2:3],
                                                      op=ALU.subtract), r=["lam_e"], w=["lam_d"])
                P.op("dve", lambda e: e.tensor_scalar(out=cst[:, 5:6], in0=cst[:, 4:5], scalar1=-lam_init,
                                                      scalar2=None, op0=ALU.add), r=["lam_d"], w=["neglam"])
                neglam = cst[:, 5:6]
                NSM = 2
                scale = 64 ** -0.5
            else:
                NSM = 1
                scale = 192 ** -0.5

            def oslot(s):
                return ps[:, (4 + s // 3) * 512 + (s % 3) * 129:(4 + s // 3) * 512 + (s % 3) * 129 + 129]

            def otag(s):
                return ("psO", 4 + s // 3)

            Osb = A.get(8 * 129, F32)

            def osb(s):
                return Osb[:, s * 129:(s + 1) * 129]

            def osbtag(s):
                return ("Osb", s // 3)

            onbs = [A.get(128) for _ in range(4)]

            def head_loads(h):
                for c in range(NCH):
                    if kind == 0:
                        P.op("sp", lambda e, h=h, c=c: e.dma_start(
                            out=Kt[:, c * 2048:(c + 1) * 2048], in_=KT[h, :, c * 2048:(c + 1) * 2048]),
                            w=[("K", c)], semkey=("K", c))
                    else:
                        P.op("sp", lambda e, h=h, c=c: [
                            e.dma_start(out=Kt[:, c * 2048:(c + 1) * 2048], in_=KT[h, :, c * 2048:(c + 1) * 2048]),
                            e.dma_start(out=Kr[0:64, c * 2048:(c + 1) * 2048], in_=KR[h, :, c * 2048:(c + 1) * 2048])],
                            w=[("K", c)], semkey=("K", c), ndma=2)
                if kind == 0:
                    P.op("sp", lambda e, h=h: e.dma_start(out=Qt, in_=QT[h, :, :]), w=["Q"], semkey="Q")
                else:
                    P.op("sp", lambda e, h=h: [e.dma_start(out=Qt, in_=QT[h, :, :]),
                                               e.dma_start(out=Qr[0:64, :], in_=QR[h, :, :])],
                         w=["Q"], semkey="Q", ndma=2)

            def head_loads_v(h):
                for c in range(NCH):
                    P.op("sp", lambda e, h=h, c=c: e.dma_start(
                        out=Vv[:, c * 16:(c + 1) * 16, 0:128], in_=VS[h, :, c * 16:(c + 1) * 16, :]),
                        w=[("V", c)], semkey=("V", c))

            steps = [(h, g, kb) for h in range(NH) for g in range(NG) for kb in range(16 * g + 16)]

            def geom(g, kb):
                jb = kb - 16 * g
                j0 = 0 if jb < 0 else jb // 4
                return jb, j0, j0 * 128

            def emit_S(i):
                h, g, kb = steps[i]
                if g == 0 and kb == 0:
                    head_loads(h)
                jb, j0, q0 = geom(g, kb)
                b = i % 2
                pS = ps[:, b * 1024:(b + 1) * 1024]
                c = kb // 16
                if kind == 0:
                    P.op("pe", lambda e: [
                        e.matmul(pS[:, q0:512], lhsT=Kt[0:64, kb * 128:(kb + 1) * 128],
                                 rhs=Qt[0:64, g * 512 + q0:(g + 1) * 512], start=True, stop=True),
                        e.matmul(pS[:, 512 + q0:1024], lhsT=Kt[64:128, kb * 128:(kb + 1) * 128],
                                 rhs=Qt[64:128, g * 512 + q0:(g + 1) * 512], start=True, stop=True)],
                        r=[("K", c), "Q"], w=[("psS", b)])
                else:
                    P.op("pe", lambda e: [
                        e.matmul(pS[:, q0:512], lhsT=Kt[:, kb * 128:(kb + 1) * 128],
                                 rhs=Qt[:, g * 512 + q0:(g + 1) * 512], start=True, stop=False),
                        e.matmul(pS[:, q0:512], lhsT=Kr[0:64, kb * 128:(kb + 1) * 128],
                                 rhs=Qr[0:64, g * 512 + q0:(g + 1) * 512], start=False, stop=True)],
                        r=[("K", c), "Q"], w=[("psS", b)])

            def emit_rest(i):
                h, g, kb = steps[i]
                jb, j0, q0 = geom(g, kb)
                b = i % 2
                pS = ps[:, b * 1024:(b + 1) * 1024]
                c = kb // 16
                pt = Pt[b]
                if q0 == 0:
                    P.op("act", lambda e: e.activation(
                        out=pt[:, 0:512 * NSM], in_=pS[:, 0:512 * NSM], func=AF.Exp, scale=scale),
                        r=[("psS", b)], w=[("Pt", b)])
                else:
                    P.op("act", lambda e: [
                        e.activation(out=pt[:, m * 512 + q0:(m + 1) * 512],
                                     in_=pS[:, m * 512 + q0:(m + 1) * 512], func=AF.Exp, scale=scale)
                        for m in range(NSM)],
                        r=[("psS", b)], w=[("Pt", b)])
                if jb >= 0:
                    P.op("dve", lambda e: [
                        e.tensor_tensor(out=pt[:, m * 512 + q0:(m + 1) * 512],
                                        in0=pt[:, m * 512 + q0:(m + 1) * 512],
                                        in1=msk[:, jb * 512 + q0:(jb + 1) * 512], op=ALU.mult)
                        for m in range(NSM)],
                        r=[("Pt", b), "msk"], w=[("Pt", b)])
                mm = []
                wl = []
                started = set()
                for j in range(j0, 4):
                    for m in range(NSM):
                        s = m * 4 + j
                        bk = 4 + s // 3
                        st_ = (kb == 0) and (bk not in started)
                        started.add(bk)
                        slots_in_bank = [s2 for s2 in range(4 * NSM) if 4 + s2 // 3 == bk]
                        bank_last = max(16 * g + 4 * (s2 % 4) + 3 for s2 in slots_in_bank)
                        s_last = max(s2 for s2 in slots_in_bank if 16 * g + 4 * (s2 % 4) + 3 == bank_last)
                        sp_ = (kb == bank_last) and (s == s_last)
                        mm.append((s, m * 512 + j * 128, st_, sp_))
                        wl.append(otag(s))
                P.op("pe", lambda e: [
                    e.matmul(oslot(s), lhsT=pt[:, o_:o_ + 128], rhs=Vv[:, kb, 0:129],
                             start=st_, stop=sp_) for (s, o_, st_, sp_) in mm],
                    r=[("Pt", b), ("V", c)], w=wl)

            def group_end_vec(h, g):
                nslots = 4 * NSM
                for bi in range((nslots + 2) // 3):
                    ns_ = min(3, nslots - bi * 3)
                    P.op("dve", lambda e, bi=bi, ns_=ns_: e.tensor_copy(
                        out=Osb[:, bi * 387:bi * 387 + ns_ * 129],
                        in_=ps[:, (4 + bi) * 512:(4 + bi) * 512 + ns_ * 129]),
                        r=[("psO", 4 + bi)], w=[("Osb", bi)])
                for j in range(4):
                    O1 = osb(j)
                    onb = onbs[j]
                    if kind == 0:
                        O2 = osb(4 + j)
                        P.op("dve", lambda e, O1=O1: e.reciprocal(out=cst[:, 8:9], in_=O1[:, 128:129]),
                             r=[osbtag(j)], w=["rl1"])
                        P.op("dve", lambda e, O2=O2: e.reciprocal(out=cst[:, 9:10], in_=O2[:, 128:129]),
                             r=[osbtag(4 + j)], w=["rl2"])
                        P.op("dve", lambda e: e.tensor_tensor(out=cst[:, 10:11], in0=cst[:, 9:10], in1=neglam,
                                                              op=ALU.mult), r=["rl2", "neglam"], w=["rl2m"])
                        P.op("dve", lambda e, O1=O1: e.tensor_scalar(out=o1, in0=O1[:, 0:128], scalar1=cst[:, 8:9],
                                                                     scalar2=None, op0=ALU.mult),
                             r=[osbtag(j), "rl1"], w=["o1"])
                        P.op("dve", lambda e, O2=O2: e.scalar_tensor_tensor(
                            out=oo, in0=O2[:, 0:128], scalar=cst[:, 10:11], in1=o1, op0=ALU.mult, op1=ALU.add),
                            r=[osbtag(4 + j), "rl2m", "o1"], w=["oo"])
                        P.op("act", lambda e: e.activation(out=junk[:, 0:128], in_=oo, func=AF.Square,
                                                           accum_out=cst[:, 11:12]),
                             r=["oo"], w=["junk", "oss"])
                        P.op("act", lambda e: e.activation(out=cst[:, 12:13], in_=cst[:, 11:12], func=AF.Ln,
                                                           scale=1.0 / 128, bias=EPS), r=["oss"], w=["oln"])
                        P.op("act", lambda e: e.activation(out=cst[:, 13:14], in_=cst[:, 12:13], func=AF.Exp,
                                                           scale=-0.5), r=["oln"], w=["ors"])
                        P.op("dve", lambda e, onb=onb: e.scalar_tensor_tensor(
                            out=onb, in0=oo, scalar=cst[:, 13:14], in1=sg, op0=ALU.mult, op1=ALU.mult),
                            r=["oo", "ors", "sg"], w=[("onb", j)])
                    else:
                        P.op("dve", lambda e, O1=O1: e.reciprocal(out=cst[:, 8:9], in_=O1[:, 128:129]),
                             r=[osbtag(j)], w=["rl1"])
                        P.op("dve", lambda e, O1=O1, onb=onb: e.tensor_scalar(
                            out=onb, in0=O1[:, 0:128], scalar1=cst[:, 8:9], scalar2=None, op0=ALU.mult),
                            r=[osbtag(j), "rl1"], w=[("onb", j)])

            def group_end_pe(h, g):
                pT = bank_bf(7)[:, 0:512]
                P.op("pe", lambda e: [e.transpose(out=pT[:, j * 128:(j + 1) * 128], in_=onbs[j], identity=identb)
                                      for j in range(4)],
                     r=[("onb", j) for j in range(4)] + ["const"], w=[("ps", 7)])
                P.op("act", lambda e: e.activation(out=attT, in_=pT, func=AF.Copy), r=[("ps", 7)], w=["attT"])
                P.op("pool", lambda e: e.dma_start(out=AT[h, :, g * 512:(g + 1) * 512], in_=attT),
                     r=["attT"], semkey="ast")

            DEFER = 8
            pending = []
            emit_S(0)
            head_loads_v(0)
            for i in range(len(steps)):
                if i + 1 < len(steps):
                    emit_S(i + 1)
                emit_rest(i)
                if i + 1 < len(steps) and steps[i + 1][1] == 0 and steps[i + 1][2] == 0:
                    head_loads_v(steps[i + 1][0])
                h, g, kb = steps[i]
                if kb == 16 * g + 15:
                    group_end_vec(h, g)
                    pending.append((i + DEFER, h, g))
                while pending and pending[0][0] <= i:
                    _, h_, g_ = pending.pop(0)
                    group_end_pe(h_, g_)
            for (_, h_, g_) in pending:
                group_end_pe(h_, g_)

            P.barrier()
            A.reset()

            NSB = ST // 128
            NTS = ST // 512
            acc = A.get(NSB * 1024, F32)
            h2T = [A.get(ST) for _ in range(8)]
            comb = A.get(NSB * 64, F32)
            Wo = A.get(8 * 1024)
            Wov = Wo.rearrange("p (h n) -> p h n", h=8)
            wrt = A.get(8 * 72, F32)
            wrv = wrt.rearrange("p (c n) -> p c n", c=8)
            brt = A.get(72, F32)
            stgD = [A.get(2048, F32) for _ in range(3)]
            Wg = [A.get(2048), A.get(2048)]
            Wu = [A.get(2048), A.get(2048)]
            Wd = [A.get(2048), A.get(2048)]
            attl = A.get(8 * 512)
            attv = attl.rearrange("p (h n) -> p h n", h=8)
            xot = A.get(4096, F32)
            h2 = xot
            h2Tf = [A.get(512, F32) for _ in range(8)]
            st4 = A.get(16, F32)
            lg = A.get(72, F32)
            rt = A.get(8 * 64 // 2, F32)
            sgt = [A.get(512, F32), A.get(512, F32)]
            hid = [[A.get(512), A.get(512)], [A.get(512), A.get(512)]]
            cr = A.get(16, F32)

            load_cast_weight(Wo, "Wo", w_out, 8, 1024, stgD[0:2], "stgD")
            P.op("sp", lambda e: [e.dma_start(out=wrv, in_=wr.rearrange("(c p) n -> p c n", p=128)),
                                  e.dma_start(out=brt, in_=br.partition_broadcast(128))],
                 w=["wr"], semkey="wr", ndma=2)

            def router_block(psl, pb, blk):
                gl = lg[:, 0:8]
                el = lg[:, 8:72]
                R = lambda a, b_: rt[:, a:b_]
                ml, oh1, ml2, oh2, pen, ohg, eg = R(0, 64), R(64, 128), R(128, 192), R(192, 256), cr[:, 8:16], None, small[:, 0:8]
                P.op("dve", lambda e: e.tensor_tensor(out=lg, in0=psl, in1=brt, op=ALU.add),
                     r=[("ps", pb), "wr"], w=["lg"])
                P.op("dve", lambda e: e.reduce_max(out=cr[:, 0:1], in_=gl, axis=AX.X), r=["lg"], w=["gmax"])
                P.op("dve", lambda e: e.tensor_scalar(out=cr[:, 1:2], in0=cr[:, 0:1], scalar1=-1.0, scalar2=None,
                                                      op0=ALU.mult), r=["gmax"], w=["ngmax"])
                P.op("act", lambda e: e.activation(out=eg, in_=gl, func=AF.Exp, bias=cr[:, 1:2], scale=1.0,
                                                   accum_out=cr[:, 2:3]),
                     r=["lg", "ngmax"], w=["eg", "gsum"])
                P.op("dve", lambda e: e.reciprocal(out=cr[:, 3:4], in_=cr[:, 2:3]), r=["gsum"], w=["gw"])
                P.op("dve", lambda e: e.tensor_scalar(out=pen, in0=gl, scalar1=cr[:, 0:1], scalar2=None,
                                                      op0=ALU.is_ge), r=["lg", "gmax"], w=["pen0"])
                P.op("dve", lambda e: e.tensor_scalar(out=pen, in0=pen, scalar1=-1.0, scalar2=1e30,
                                                      op0=ALU.add, op1=ALU.mult), r=["pen0"], w=["pen"])
                for gi in range(8):
                    P.op("dve", lambda e, gi=gi: e.tensor_scalar(
                        out=ml[:, gi * 8:(gi + 1) * 8], in0=el[:, gi * 8:(gi + 1) * 8],
                        scalar1=pen[:, gi:gi + 1], scalar2=None, op0=ALU.add),
                        r=["lg", "pen"], w=[("ml", gi)])
                mlr = [("ml", gi) for gi in range(8)]
                P.op("dve", lambda e: e.reduce_max(out=cr[:, 4:5], in_=ml, axis=AX.X), r=mlr, w=["m1"])
                P.op("dve", lambda e: e.tensor_scalar(out=oh1, in0=ml, scalar1=cr[:, 4:5], scalar2=None,
                                                      op0=ALU.is_ge), r=mlr + ["m1"], w=["oh1"])
                P.op("dve", lambda e: e.scalar_tensor_tensor(out=ml2, in0=oh1, scalar=-1e30, in1=ml,
                                                             op0=ALU.mult, op1=ALU.add),
                     r=mlr + ["oh1"], w=["ml2"])
                P.op("dve", lambda e: e.reduce_max(out=cr[:, 5:6], in_=ml2, axis=AX.X), r=["ml2"], w=["m2"])
                P.op("dve", lambda e: e.tensor_scalar(out=oh2, in0=ml2, scalar1=cr[:, 5:6], scalar2=None,
                                                      op0=ALU.is_ge), r=["ml2", "m2"], w=["oh2"])
                P.op("dve", lambda e: e.tensor_tensor(out=cr[:, 6:7], in0=cr[:, 5:6], in1=cr[:, 4:5],
                                                      op=ALU.subtract), r=["m1", "m2"], w=["dd"])
                P.op("act", lambda e: e.activation(out=cr[:, 7:8], in_=cr[:, 6:7], func=AF.Exp), r=["dd"], w=["ed"])
                P.op("dve", lambda e: e.tensor_scalar(out=cr[:, 6:7], in0=cr[:, 7:8], scalar1=1.0, scalar2=None,
                                                      op0=ALU.add), r=["ed"], w=["den"])
                P.op("dve", lambda e: e.reciprocal(out=cr[:, 7:8], in_=cr[:, 6:7]), r=["den"], w=["rden"])
                P.op("dve", lambda e: e.tensor_tensor(out=cr[:, 6:7], in0=cr[:, 7:8], in1=cr[:, 3:4], op=ALU.mult),
                     r=["rden", "gw"], w=["w1"])
                P.op("dve", lambda e: e.tensor_tensor(out=cr[:, 7:8], in0=cr[:, 3:4], in1=cr[:, 6:7],
                                                      op=ALU.subtract), r=["w1", "gw"], w=["w2"])
                cb = comb[:, blk * 64:(blk + 1) * 64]
                P.op("dve", lambda e: e.tensor_scalar(out=cb, in0=oh1, scalar1=cr[:, 6:7], scalar2=None,
                                                      op0=ALU.mult), r=["oh1", "w1"], w=[("comb", blk)])
                P.op("dve", lambda e: e.scalar_tensor_tensor(out=cb, in0=oh2, scalar=cr[:, 7:8], in1=cb,
                                                             op0=ALU.mult, op1=ALU.add),
                     r=["oh2", "w2", ("comb", blk)], w=[("comb", blk)])

            for sti in range(SO // ST):
                for tt in range(NTS):
                    t = sti * NTS + tt
                    P.op("sp", lambda e, t=t: e.dma_start(
                        out=attv, in_=AT[:, :, t * 512:(t + 1) * 512].rearrange("h p n -> p h n")),
                        w=["attl"], semkey="attl")
                    load_x_tile(xo, t, xot, "xot", extra_w=[("h2", j_) for j_ in range(4)])
                    for j in range(4):
                        blk = tt * 4 + j
                        for half in range(2):
                            pb = half
                            P.op("pe", lambda e, j=j, half=half, pb=pb: [
                                e.matmul(bank(pb), lhsT=attv[:, hh, j * 128:(j + 1) * 128],
                                         rhs=Wov[:, hh, half * 512:(half + 1) * 512],
                                         start=(hh == 0), stop=(hh == 7)) for hh in range(8)],
                                r=["attl"] + [("Wo", c) for c in range(8)], w=[("ps", pb)])
                            P.op("dve", lambda e, j=j, half=half, pb=pb, blk=blk: e.tensor_tensor(
                                out=acc[:, blk * 1024 + half * 512:blk * 1024 + (half + 1) * 512],
                                in0=bank(pb), in1=xot[:, j * 1024 + half * 512:j * 1024 + (half + 1) * 512],
                                op=ALU.add), r=[("ps", pb), "xot"], w=[("acc", blk, half)])
                        P.op("act", lambda e, j=j, blk=blk: e.activation(
                            out=junk, in_=acc[:, blk * 1024:(blk + 1) * 1024], func=AF.Square,
                            accum_out=st4[:, j:j + 1]),
                            r=[("acc", blk, 0), ("acc", blk, 1)], w=["junk", ("st4", j)])
                    P.op("act", lambda e: e.activation(out=st4[:, 4:8], in_=st4[:, 0:4], func=AF.Ln,
                                                       scale=1.0 / D, bias=EPS),
                         r=[("st4", j) for j in range(4)], w=[("st4", "ln")])
                    P.op("act", lambda e: e.activation(out=st4[:, 8:12], in_=st4[:, 4:8], func=AF.Exp, scale=-0.5),
                         r=[("st4", "ln")], w=[("st4", "rs")])
                    for j in range(4):
                        blk = tt * 4 + j
                        P.op("act", lambda e, j=j, blk=blk: e.activation(
                            out=h2[:, j * 1024:(j + 1) * 1024], in_=acc[:, blk * 1024:(blk + 1) * 1024],
                            func=AF.Copy, scale=st4[:, 8 + j:9 + j]),
                            r=[("acc", blk, 0), ("acc", blk, 1), ("st4", "rs")], w=[("h2", j)])
                    for c in range(8):
                        pb = 2 + c % 2
                        P.op("pe", lambda e, c=c, pb=pb: [
                            e.transpose(out=bank(pb)[:, j * 128:(j + 1) * 128],
                                        in_=h2[:, j * 1024 + c * 128:j * 1024 + (c + 1) * 128], identity=identf)
                            for j in range(4)],
                            r=[("h2", j) for j in range(4)] + ["const"], w=[("ps", pb)])
                        P.op("act", lambda e, c=c, pb=pb: e.activation(
                            out=h2Tf[c], in_=bank(pb), func=AF.Copy, scale=gff[:, c:c + 1]),
                            r=[("ps", pb), "const"], w=[("h2Tf", c)])
                        P.op("dve", lambda e, c=c, tt=tt: e.tensor_copy(
                            out=h2T[c][:, tt * 512:(tt + 1) * 512], in_=h2Tf[c]),
                            r=[("h2Tf", c)], w=[("h2T", c, tt)])
                    for j in range(4):
                        blk = tt * 4 + j
                        pb = 4 + j % 2
                        P.op("pe", lambda e, j=j, pb=pb: [
                            e.matmul(bank(pb)[:, 0:72], lhsT=h2Tf[c][:, j * 128:(j + 1) * 128], rhs=wrv[:, c, :],
                                     start=(c == 0), stop=(c == 7)) for c in range(8)],
                            r=[("h2Tf", c) for c in range(8)] + ["wr"], w=[("ps", pb)])
                        router_block(bank(pb)[:, 0:72], pb, blk)
                pend = []
                item_no = [0]
                for ex in range(NEXP):
                    sl = ex % 2
                    for (wsrc, wdst, wtag, si) in ((w_gate, Wg, "Wg", 0), (w_up, Wu, "Wu", 1)):
                        P.op("sp", lambda e, wsrc=wsrc, ex=ex, si=si: e.dma_start(
                            out=stgD[si].rearrange("p (c n) -> p c n", c=8),
                            in_=wsrc[ex].rearrange("(c p) n -> p c n", p=128)),
                            w=[("stgD", si)], semkey=("stgD", si))
                        P.op("pool", lambda e, wdst=wdst, si=si, sl=sl: e.tensor_copy(out=wdst[sl], in_=stgD[si]),
                             r=[("stgD", si)], w=[(wtag, sl)])
                    P.op("sp", lambda e, ex=ex: e.dma_start(
                        out=stgD[2].rearrange("p (c n) -> p c n", c=2),
                        in_=w_down[ex].rearrange("(c p) n -> p c n", p=128)),
                        w=[("stgD", 2)], semkey=("stgD", 2))
                    P.op("pool", lambda e, sl=sl: e.tensor_copy(out=Wd[sl], in_=stgD[2]),
                         r=[("stgD", 2)], w=[("Wd", sl)])
                    Wgv = Wg[sl].rearrange("p (c n) -> p c n", c=8)
                    Wuv = Wu[sl].rearrange("p (c n) -> p c n", c=8)
                    Wdv = Wd[sl].rearrange("p (c n) -> p c n", c=2)

                    def down_chunks(tt, par, ex=ex, sl=sl, Wdv=Wdv):
                        hs = hid[par]
                        out = []
                        for j in range(4):
                            def chunk(j=j, tt=tt, par=par, hs=hs):
                                blk = tt * 4 + j
                                for half in range(2):
                                    pb = 4 + (j * 2 + half) % 4
                                    P.op("pe", lambda e, j=j, half=half, pb=pb, hs=hs: [
                                        e.matmul(bank(pb), lhsT=hs[f][:, j * 128:(j + 1) * 128],
                                                 rhs=Wdv[:, f, half * 512:(half + 1) * 512],
                                                 start=(f == 0), stop=(f == 1)) for f in range(2)],
                                        r=[("hid", par, 0), ("hid", par, 1), ("Wd", sl)], w=[("ps", pb)])
                                    P.op("dve", lambda e, blk=blk, half=half, pb=pb: e.scalar_tensor_tensor(
                                        out=acc[:, blk * 1024 + half * 512:blk * 1024 + (half + 1) * 512],
                                        in0=bank(pb), scalar=comb[:, blk * 64 + ex:blk * 64 + ex + 1],
                                        in1=acc[:, blk * 1024 + half * 512:blk * 1024 + (half + 1) * 512],
                                        op0=ALU.mult, op1=ALU.add),
                                        r=[("ps", pb), ("comb", blk), ("acc", blk, half)], w=[("acc", blk, half)])
                            out.append(chunk)
                        return out

                    for tt in range(NTS):
                        par = item_no[0] % 2
                        item_no[0] += 1
                        for f in range(2):
                            P.op("pe", lambda e, f=f, tt=tt, Wgv=Wgv: [
                                e.matmul(bank(f), lhsT=Wgv[:, c, f * 128:(f + 1) * 128],
                                         rhs=h2T[c][:, tt * 512:(tt + 1) * 512], start=(c == 0), stop=(c == 7))
                                for c in range(8)],
                                r=[("Wg", sl)] + [("h2T", c, tt) for c in range(8)], w=[("ps", f)])
                            if pend:
                                pend.pop(0)()
                            P.op("pe", lambda e, f=f, tt=tt, Wuv=Wuv: [
                                e.matmul(bank(2 + f), lhsT=Wuv[:, c, f * 128:(f + 1) * 128],
                                         rhs=h2T[c][:, tt * 512:(tt + 1) * 512], start=(c == 0), stop=(c == 7))
                                for c in range(8)],
                                r=[("Wu", sl)] + [("h2T", c, tt) for c in range(8)], w=[("ps", 2 + f)])
                            if pend:
                                pend.pop(0)()
                            P.op("act", lambda e, f=f: e.activation(out=sgt[f], in_=bank(f), func=AF.Silu),
                                 r=[("ps", f)], w=[("sgt", f)])
                            P.op("dve", lambda e, f=f, par=par: e.tensor_tensor(
                                out=hid[par][f], in0=sgt[f], in1=bank(2 + f), op=ALU.mult),
                                r=[("sgt", f), ("ps", 2 + f)], w=[("hid", par, f)])
                        while pend:
                            pend.pop(0)()
                        pend.extend(down_chunks(tt, par))
                while pend:
                    pend.pop(0)()
                for blk in range(NSB):
                    r0 = sti * ST + blk * 128
                    P.op("sp", lambda e, blk=blk, r0=r0: e.dma_start(
                        out=y[r0:r0 + 128, :], in_=acc[:, blk * 1024:(blk + 1) * 1024]),
                        r=[("acc", blk, 0), ("acc", blk, 1)], semkey=("yout", blk))
            P.barrier()
            A.reset()

        emit_layer(0, xb_in, False, xo_in, X1)
        for k in range(SO // CC_ROWS):
            P.op("pool", lambda e, k=k: e.collective_compute(
                "AllGather", ALU.bypass, replica_groups=[[0, 1, 2, 3], [4, 5, 6, 7]],
                ins=[X1[k * CC_ROWS:(k + 1) * CC_ROWS, :].opt()],
                outs=[XG[k * 4 * CC_ROWS:(k + 1) * 4 * CC_ROWS, :].opt()]),
                w=["XG"], semkey="cc", dmainc=1)
        P.barrier()
        emit_layer(1, XG, True, X1, y_out)
        P.emit(nc, stack)
    return nc


def _own_positions(S, r):
    nb = S // 512
    return np.concatenate([np.arange((4 * i + r) * 128, (4 * i + r + 1) * 128) for i in range(nb)])


def _rope_tables(pos):
    half = 32
    inv = (1.0 / (10000.0 ** (np.arange(0, 64, 2, dtype=np.float32) / np.float32(64)))).astype(np.float32)
    ang = pos.astype(np.float32)[:, None] * inv[None, :]
    cos = np.cos(ang).astype(np.float32)
    sin = np.sin(ang).astype(np.float32)
    idx = (np.arange(128) % 64) % half
    return np.ascontiguousarray(cos[:, idx].T), np.ascontiguousarray(sin[:, idx].T)


def _consts(S):
    bf = ml_dtypes.bfloat16
    identf = np.eye(128, dtype=np.float32)
    onesbd = np.zeros((128, 128), np.float32)
    onesbd[:64, :64] = 1
    onesbd[64:, 64:] = 1
    rot = np.zeros((128, 128), np.float32)
    for dp in range(128):
        if dp % 64 < 32:
            rot[dp + 32, dp] = -1.0
        else:
            rot[dp - 32, dp] = 1.0
    return {
        "identb": identf.astype(bf), "identf": identf, "onesbd": onesbd.astype(bf),
        "ones": np.ones((128, 128), np.float32).astype(bf), "rot": rot.astype(bf),
    }


def _mask(r):
    k = np.arange(128)[:, None, None]
    jb = np.arange(16)[None, :, None]
    q = np.arange(512)[None, None, :]
    keypos = jb * 128 + k
    qpos = (r + 4 * (q // 128)) * 128 + (q % 128)
    return (keypos <= qpos).astype(np.float32).reshape(128, 16 * 512).astype(ml_dtypes.bfloat16)


_PROG_CACHE = {}


def _get_prog(S):
    if S not in _PROG_CACHE:
        _PROG_CACHE[S] = build_program(S)
    return _PROG_CACHE[S]


def _col(v, n=128):
    return np.ascontiguousarray(np.asarray(v, np.float32).reshape(-1, n).T)


def _make_in_maps(x, inp):
    B, S, _ = x.shape
    consts = _consts(S)
    cosk, sink = _rope_tables(np.arange(S))
    f32 = np.float32
    shared = dict(consts)
    shared.update({
        "cosk": cosk, "sink": sink,
        "g_attn": np.concatenate([_col(inp["attn_norm"][0]), _col(inp["attn_norm"][1])], axis=1),
        "g_ffn": np.concatenate([_col(inp["ffn_norm"][0]), _col(inp["ffn_norm"][1])], axis=1),
        "w_in": inp["diff_w_in"][0],
        "gq": np.tile(inp["diff_q_norm"][0], 2)[:, None].astype(f32),
        "gk": np.tile(inp["diff_k_norm"][0], 2)[:, None].astype(f32),
        "lam4": np.stack([inp["diff_lambda_q1"][0], inp["diff_lambda_k1"][0],
                          inp["diff_lambda_q2"][0], inp["diff_lambda_k2"][0]]).astype(f32).reshape(1, 256),
        "subln": inp["diff_subln"][0][None, :].astype(f32),
        "w_a": inp["mla_w_a"][0], "w_qb": inp["mla_w_qb"][0], "w_kvb": inp["mla_w_kvb"][0],
        "g_qa": _col(inp["mla_q_a_norm"][0]), "g_kva": _col(inp["mla_kv_a_norm"][0]),
    })
    qn, kn = inp["mla_q_norm"][0].astype(f32), inp["mla_k_norm"][0].astype(f32)
    shared.update({
        "gqn": np.ascontiguousarray(qn[:128, None]), "gqr": np.ascontiguousarray(qn[128:, None]),
        "gkn": np.ascontiguousarray(kn[:128, None]), "gkr": np.ascontiguousarray(kn[128:, None]),
    })
    wouts = (inp["diff_w_out"][0], inp["mla_w_out"][0])
    for li in range(2):
        pf = "L%d_" % li
        shared.update({
            pf + "w_out": wouts[li],
            pf + "wr": np.ascontiguousarray(np.concatenate([inp["moe_w_group"][li], inp["moe_w_expert"][li]], axis=1)),
            pf + "br": np.concatenate([inp["moe_b_group"][li], inp["moe_b_expert"][li]])[None, :].astype(f32),
            pf + "w_gate": inp["moe_w_gate"][li], pf + "w_up": inp["moe_w_up"][li],
            pf + "w_down": inp["moe_w_down"][li],
        })
    in_maps, owns = [], []
    for c in range(NCORES):
        b, r = c // 4, c % 4
        own = _own_positions(S, r)
        owns.append((b, own))
        cosq, sinq = _rope_tables(own)
        m = dict(shared)
        m.update({"xb": np.ascontiguousarray(x[b]), "xo": np.ascontiguousarray(x[b][own]),
                  "cosq": cosq, "sinq": sinq, "maskd": _mask(r)})
        in_maps.append(m)
    return in_maps, owns


def run_fused(x, inp):
    B, S, _ = x.shape
    nc = _get_prog(S)
    in_maps, owns = _make_in_maps(x, inp)
    res = run_bass_kernel_spmd(nc, in_maps, core_ids=list(range(NCORES)))
    out = np.empty_like(x)
    for c in range(NCORES):
        b, own = owns[c]
        out[b][own] = res.results[c]["y"]
    return out


def kernel(**inputs):
    inp = {k: np.asarray(v) for k, v in inputs.items()}
    x = np.ascontiguousarray(inp["x"], dtype=np.float32)
    return run_fused(x, inp)
```
